# Optimizing a Trainium2 kernel written in Bass

```python
import jax, jax.numpy as jnp
from jax import lax
import numpy as np

D_MODEL = 1024
BATCH = 16
SEQ = 2048
DEPTH = 2

CHUNK = 64
D_MIX = D_MODEL
C_CONV = D_MIX // 4
CONV_WIDTH = 31
N_HEADS = 8
HEAD_DIM = 64
D_ATT = N_HEADS * HEAD_DIM
D_Q_LAT = 256
D_KV_LAT = 128
IDX_HEADS = 8
IDX_DIM = 64
TOPK_MAX = 256
Q_BLOCK = 128
C_POOL = D_MIX // 4
POOL_WINDOWS = (2, 4, 8, 16)
POOL_GROUP = C_POOL // 4
N_GROUPS = 4
EXPERTS_PER_GROUP = 8
N_EXPERTS = N_GROUPS * EXPERTS_PER_GROUP
D_EXPERT = 512
MOE_BLOCK = 128
EPS = 1e-6
IN_SPLITS = (C_CONV, C_CONV, D_Q_LAT, D_KV_LAT, IDX_HEADS * IDX_DIM, IDX_DIM, IDX_HEADS, C_POOL)
N_IN = sum(IN_SPLITS)

kernel_name = 'hybrid_chunk_causal_conv_dsa_pool_hmoe'


def _rmsnorm(x, g):
    xf = x.astype(jnp.float32)
    y = xf * lax.rsqrt(jnp.mean(xf * xf, axis=-1, keepdims=True) + EPS)
    return y.astype(x.dtype) * g


def _layernorm(x, g, b):
    xf = x.astype(jnp.float32)
    mu = jnp.mean(xf, axis=-1, keepdims=True)
    var = jnp.mean(jnp.square(xf - mu), axis=-1, keepdims=True)
    return ((xf - mu) * lax.rsqrt(var + EPS)).astype(x.dtype) * g + b


def _conv_module(a_val, a_gate, conv_k, conv_b, ln_g, ln_b):
    u = a_val * jax.nn.sigmoid(a_gate)
    u = lax.conv_general_dilated(
        u, conv_k[:, None, :].astype(u.dtype), window_strides=(1,),
        padding=[(CONV_WIDTH - 1, 0)], dimension_numbers=('NWC', 'WIO', 'NWC'),
        feature_group_count=C_CONV) + conv_b
    return jax.nn.silu(_layernorm(u, ln_g, ln_b))


def _dsa_attention(cq, ckv, qi, ki, wi, q_norm_g, kv_norm_g, w_uq, w_uk, w_uv):
    B, S, _ = ckv.shape
    topk = min(TOPK_MAX, S // 4)
    nb = S // Q_BLOCK
    cq = _rmsnorm(cq, q_norm_g)
    ckv = _rmsnorm(ckv, kv_norm_g)
    q = jnp.einsum('btc,chd->bthd', cq, w_uq)
    q_abs = jnp.einsum('bthd,khd->bthk', q, w_uk)
    qi = qi.reshape(B, S, IDX_HEADS, IDX_DIM)
    key_chunk = jnp.arange(S) // CHUNK
    idx_scale = (IDX_HEADS * IDX_DIM) ** -0.5
    att_scale = HEAD_DIM ** -0.5

    def to_blocks(a):
        return jnp.moveaxis(a.reshape(B, nb, Q_BLOCK, *a.shape[2:]), 1, 0)

    def block(args):
        qa, qib, wib, bi = args
        qchunk = (bi * Q_BLOCK + jnp.arange(Q_BLOCK)) // CHUNK
        allowed = key_chunk[None, :] <= qchunk[:, None]
        rel = jax.nn.relu(jnp.einsum('bqhd,bsd->bqhs', qib, ki).astype(jnp.float32))
        score = jnp.einsum('bqhs,bqh->bqs', rel, wib.astype(jnp.float32)) * idx_scale
        score = jnp.where(allowed[None], score, -jnp.inf)
        _, idx = lax.top_k(score, topk)
        kv_sel = jax.vmap(lambda a, i: a[i])(ckv, idx)
        logits = jnp.einsum('bqhc,bqkc->bqhk', qa, kv_sel).astype(jnp.float32) * att_scale
        valid = (idx // CHUNK) <= qchunk[None, :, None]
        logits = jnp.where(valid[:, :, None, :], logits, -jnp.inf)
        p = jax.nn.softmax(logits, axis=-1).astype(kv_sel.dtype)
        return jnp.einsum('bqhk,bqkc->bqhc', p, kv_sel)

    ctx = lax.map(block, (to_blocks(q_abs), to_blocks(qi), to_blocks(wi), jnp.arange(nb)))
    ctx = jnp.moveaxis(ctx, 0, 1).reshape(B, S, N_HEADS, D_KV_LAT)
    o = jnp.einsum('bthc,chd->bthd', ctx, w_uv)
    return o.reshape(B, S, D_ATT)


def _pool_mixer(u, pool_w, pool_scale):
    B, S, _ = u.shape
    n_pos = jnp.arange(1, S + 1, dtype=jnp.float32)
    outs = []
    for g, w in enumerate(POOL_WINDOWS):
        ug = u[..., g * POOL_GROUP:(g + 1) * POOL_GROUP]
        ugf = ug.astype(jnp.float32)
        cs = jnp.cumsum(ugf, axis=1)
        prev = jnp.pad(cs, ((0, 0), (w, 0), (0, 0)))[:, :S]
        mean = (cs - prev) / jnp.minimum(n_pos, w)[None, :, None]
        outs.append((mean - ugf).astype(u.dtype) @ pool_w[g])
    return jnp.concatenate(outs, axis=-1) * pool_scale


def _hier_moe(h, rg_w, rg_b, re_w, re_b, w1, w3, w2):
    B, S, D = h.shape
    T = B * S
    A = 2 * T
    hf = h.reshape(T, D)
    g_prob = jax.nn.softmax((hf @ rg_w).astype(jnp.float32) + rg_b, axis=-1)
    g_p, g_idx = lax.top_k(g_prob, 1)
    e_logits = jnp.einsum('td,dge->tge', hf, re_w).astype(jnp.float32) + re_b
    e_sel = jnp.take_along_axis(e_logits, g_idx[:, :, None], axis=1)[:, 0]
    e_p, e_idx = lax.top_k(jax.nn.softmax(e_sel, axis=-1), 2)
    e_p = e_p / jnp.sum(e_p, axis=-1, keepdims=True)
    gates = (g_p * e_p).reshape(A)
    a_exp = (g_idx * EXPERTS_PER_GROUP + e_idx).reshape(A).astype(jnp.int32)
    a_tok = jnp.repeat(jnp.arange(T, dtype=jnp.int32), 2)
    order = jnp.argsort(a_exp)
    s_exp = a_exp[order]
    counts = jnp.bincount(a_exp, length=N_EXPERTS)
    starts = jnp.cumsum(counts) - counts
    pcounts = (counts + MOE_BLOCK - 1) // MOE_BLOCK * MOE_BLOCK
    pends = jnp.cumsum(pcounts)
    pstarts = pends - pcounts
    dst = pstarts[s_exp] + jnp.arange(A) - starts[s_exp]
    n_blk = -(-(A + N_EXPERTS * (MOE_BLOCK - 1)) // MOE_BLOCK)
    P = n_blk * MOE_BLOCK
    slot_tok = jnp.zeros((P,), jnp.int32).at[dst].set(a_tok[order])
    slot_gate = jnp.zeros((P,), jnp.float32).at[dst].set(gates[order])
    blk_exp = jnp.minimum(jnp.searchsorted(pends, jnp.arange(n_blk) * MOE_BLOCK, side='right'), N_EXPERTS - 1)

    def run(args):
        tok, e = args
        hb = hf[tok]
        return (jax.nn.silu(hb @ w1[e]) * (hb @ w3[e])) @ w2[e]

    y = lax.map(run, (slot_tok.reshape(n_blk, MOE_BLOCK), blk_exp)).reshape(P, D)
    out = jnp.zeros((T, D), h.dtype).at[slot_tok].add(y * slot_gate[:, None].astype(y.dtype))
    return out.reshape(B, S, D)


def setup_inputs(seed: int = 0) -> dict:
    key = jax.random.key(seed)
    ks = jax.random.split(key, 27)

    def nrm(k, shape, scale):
        return jax.random.normal(k, shape, jnp.float32) * scale

    L = DEPTH
    return {
        'x': nrm(ks[0], (BATCH, SEQ, D_MODEL), 1.0),
        'c': nrm(ks[1], (BATCH, D_MODEL), 1.0),
        'mod_w': nrm(ks[2], (L, D_MODEL, 6 * D_MODEL), 0.5 * D_MODEL ** -0.5),
        'mod_b': nrm(ks[3], (L, 6 * D_MODEL), 0.1),
        'norm1_g': 1.0 + nrm(ks[4], (L, D_MODEL), 0.05),
        'w_in': nrm(ks[5], (L, D_MODEL, N_IN), D_MODEL ** -0.5),
        'conv_k': nrm(ks[6], (L, CONV_WIDTH, C_CONV), CONV_WIDTH ** -0.5),
        'conv_b': nrm(ks[7], (L, C_CONV), 0.02),
        'conv_ln_g': 1.0 + nrm(ks[8], (L, C_CONV), 0.05),
        'conv_ln_b': nrm(ks[9], (L, C_CONV), 0.02),
        'q_norm_g': 1.0 + nrm(ks[10], (L, D_Q_LAT), 0.05),
        'kv_norm_g': 1.0 + nrm(ks[11], (L, D_KV_LAT), 0.05),
        'w_uq': nrm(ks[12], (L, D_Q_LAT, N_HEADS, HEAD_DIM), D_Q_LAT ** -0.5),
        'w_uk': nrm(ks[13], (L, D_KV_LAT, N_HEADS, HEAD_DIM), D_KV_LAT ** -0.5),
        'w_uv': nrm(ks[14], (L, D_KV_LAT, N_HEADS, HEAD_DIM), D_KV_LAT ** -0.5),
        'pool_w': nrm(ks[15], (L, len(POOL_WINDOWS), POOL_GROUP, POOL_GROUP), POOL_GROUP ** -0.5),
        'pool_scale': 1.0 + nrm(ks[16], (L, C_POOL), 0.1),
        'w_out': nrm(ks[17], (L, D_MIX, D_MODEL), D_MIX ** -0.5),
        'norm2_g': 1.0 + nrm(ks[18], (L, D_MODEL), 0.05),
        'router_g_w': nrm(ks[19], (L, D_MODEL, N_GROUPS), D_MODEL ** -0.5),
        'router_g_b': nrm(ks[20], (L, N_GROUPS), 0.01),
        'router_e_w': nrm(ks[21], (L, D_MODEL, N_GROUPS, EXPERTS_PER_GROUP), D_MODEL ** -0.5),
        'router_e_b': nrm(ks[22], (L, N_GROUPS, EXPERTS_PER_GROUP), 0.01),
        'exp_w1': nrm(ks[23], (L, N_EXPERTS, D_MODEL, D_EXPERT), D_MODEL ** -0.5),
        'exp_w3': nrm(ks[24], (L, N_EXPERTS, D_MODEL, D_EXPERT), D_MODEL ** -0.5),
        'exp_w2': nrm(ks[25], (L, N_EXPERTS, D_EXPERT, D_MODEL), D_EXPERT ** -0.5),
        'final_g': 1.0 + nrm(ks[26], (D_MODEL,), 0.05),
    }


def reference(x, c, mod_w, mod_b, norm1_g, w_in, conv_k, conv_b, conv_ln_g, conv_ln_b,
              q_norm_g, kv_norm_g, w_uq, w_uk, w_uv, pool_w, pool_scale, w_out, norm2_g,
              router_g_w, router_g_b, router_e_w, router_e_b, exp_w1, exp_w3, exp_w2, final_g):
    cond = jax.nn.silu(c)
    split_at = np.cumsum(IN_SPLITS)[:-1].tolist()
    for l in range(DEPTH):
        mod = cond @ mod_w[l] + mod_b[l]
        sh1, sc1, g1, sh2, sc2, g2 = [m[:, None, :] for m in jnp.split(mod, 6, axis=-1)]
        h = _rmsnorm(x, norm1_g[l]) * (1 + sc1) + sh1
        z = h @ w_in[l]
        a_val, a_gate, cq, ckv, qi, ki, wi, u_pool = jnp.split(z, split_at, axis=-1)
        y_conv = _conv_module(a_val, a_gate, conv_k[l], conv_b[l], conv_ln_g[l], conv_ln_b[l])
        y_att = _dsa_attention(cq, ckv, qi, ki, wi, q_norm_g[l], kv_norm_g[l], w_uq[l], w_uk[l], w_uv[l])
        y_pool = _pool_mixer(u_pool, pool_w[l], pool_scale[l])
        mix = jnp.concatenate([y_conv, y_att, y_pool], axis=-1) @ w_out[l]
        x = x + g1 * mix
        h = _rmsnorm(x, norm2_g[l]) * (1 + sc2) + sh2
        x = x + g2 * _hier_moe(h, router_g_w[l], router_g_b[l], router_e_w[l], router_e_b[l],
                               exp_w1[l], exp_w3[l], exp_w2[l])
    return _rmsnorm(x, final_g)
```

```python
import numpy as np
import ml_dtypes
from contextlib import ExitStack
import concourse.bass as bass
import concourse.mybir as mybir
from concourse.bass_utils import run_bass_kernel_spmd

F32 = mybir.dt.float32
BF16 = mybir.dt.bfloat16
I32 = mybir.dt.int32
U32 = mybir.dt.uint32
AF = mybir.ActivationFunctionType
ALU = mybir.AluOpType
AX = mybir.AxisListType

NCORES = 8
D = 1024
SEQ = 2048
NSEQ = 2
T = NSEQ * SEQ
NT = T // 128
DEPTH = 2
N_IN = 1736
EPS = 1e-6
NEXP = 32
DEXP = 512
TOPK = 256
MB = 256
NBLK = (2 * T + NEXP * (MB - 1)) // MB + 1
PSLOT = NBLK * MB


class Sched:
    def __init__(self, nc, es, ndma=8, same_engine_sync=True):
        self.nc = nc
        self.E = {'pe': nc.tensor, 'act': nc.scalar, 'dve': nc.vector, 'pool': nc.gpsimd, 'sp': nc.sync}
        self.same = same_engine_sync
        self.csem = {}
        self.ccnt = {}
        for e in ['pe', 'act', 'dve', 'pool']:
            self.csem[e] = es.enter_context(nc.semaphore(f"c_{e}"))
            self.ccnt[e] = 0
        self.dsem = {}
        self.dtot = {}
        self.dnext = {}
        for q in ['sp', 'pool', 'act']:
            self.dsem[q] = [es.enter_context(nc.semaphore(f"d_{q}_{i}")) for i in range(ndma)]
            self.dtot[q] = [0] * ndma
            self.dnext[q] = 0
        self.waited = {e: {} for e in self.E}
        self.res = {}
        self.nwait = 0
        self.nins = 0

    @staticmethod
    def _key(x):
        return x if isinstance(x, (str, tuple)) else x.name

    def _wait(self, eng, tok):
        sem, sname, val, src = tok
        if src == eng:
            if eng == 'pe' or not self.same:
                return
        if self.waited[eng].get(sname, 0) >= val:
            return
        self.E[eng].wait_ge(sem, val)
        self.waited[eng][sname] = val
        self.nwait += 1

    def _deps(self, eng, r, w):
        for k in r:
            st = self.res.get(self._key(k))
            if st and st[0] is not None:
                self._wait(eng, st[0])
        for k in w:
            st = self.res.get(self._key(k))
            if st:
                if st[0] is not None:
                    self._wait(eng, st[0])
                for t in st[1].values():
                    self._wait(eng, t)

    def _commit(self, tok, r, w):
        for k in w:
            self.res[self._key(k)] = [tok, {}]
        for k in r:
            st = self.res.setdefault(self._key(k), [None, {}])
            st[1][tok[1]] = tok

    def op(self, eng, fn, r=(), w=()):
        self._deps(eng, r, w)
        ins = fn(self.E[eng])
        self.ccnt[eng] += 1
        ins.then_inc(self.csem[eng], 1)
        tok = (self.csem[eng], 'c_' + eng, self.ccnt[eng], eng)
        self._commit(tok, r, w)
        self.nins += 1
        return tok

    def dma(self, q, fn, r=(), w=()):
        self._deps(q, r, w)
        i = self.dnext[q]
        self.dnext[q] = (i + 1) % len(self.dsem[q])
        sem = self.dsem[q][i]
        sname = f"d_{q}_{i}"
        if self.dtot[q][i] > 0:
            self._wait(q, (sem, sname, self.dtot[q][i], 'dma'))
        ins = fn(self.E[q])
        self.dtot[q][i] += 16
        ins.then_inc(sem, 16)
        tok = (sem, sname, self.dtot[q][i], 'dma')
        self._commit(tok, r, w)
        self.nins += 1
        return tok

    def raw(self, eng, r=(), w=()):
        self._deps(eng, r, w)

    def barrier(self):
        toks = []
        for e, s in self.csem.items():
            if self.ccnt[e]:
                toks.append((s, 'c_' + e, self.ccnt[e], 'x'))
        for q in self.dsem:
            for i, s in enumerate(self.dsem[q]):
                if self.dtot[q][i]:
                    toks.append((s, f"d_{q}_{i}", self.dtot[q][i], 'dma'))
        for e in self.E:
            for t in toks:
                self._wait(e, t)
        self.res = {}


def _bf16(a):
    return np.asarray(a, dtype=np.float32).astype(ml_dtypes.bfloat16)


def make_consts():
    cf = np.zeros((128, 1024), np.float32)
    cf[:, 0:128] = np.eye(128)
    cf[:, 128:256] = 1.0
    cf[:, 256] = np.arange(128)
    cf[0, 640:768] = 1.0
    cf[1, 768:896] = 1.0
    cf[:, 896:896 + 32] = (2.0 ** -(np.arange(32) + 1.0))[None, :]
    cf[:, 384:384 + 32] = np.arange(32)[None, :]
    cf[:, 512:512 + NBLK] = (np.arange(NBLK) * MB)[None, :]
    cb = np.zeros((128, 512), np.float32)
    cb[:, 0:128] = np.eye(128)
    cb[:, 128:256] = 1.0
    cb[:, 256:384] = np.triu(np.ones((128, 128)), 1)
    inv = np.zeros((128, 2, SEQ), np.float32)
    n = np.arange(1, SEQ + 1, dtype=np.float32)
    for g, wdw in enumerate((2, 4, 8, 16)):
        ch, half = divmod(g, 2)
        inv[half * 64:(half + 1) * 64, ch, :] = (1.0 / np.minimum(n, wdw))[None, :]
    return cf, _bf16(cb), inv


class Prog:
    def __init__(self, nlayers=DEPTH, phases=None, debug=False, same_engine_sync=True):
        self.nlayers = nlayers
        self.debug = debug
        self.phases = phases
        self.nc = nc = bass.Bass("TRN2", target_bir_lowering=False)
        self.es = ExitStack()
        self.S = Sched(nc, self.es, same_engine_sync=same_engine_sync)
        self.inp = {}
        self.outs = []

    def din(self, name, shape, dt=F32):
        t = self.nc.dram_tensor(name, list(shape), dt, kind="ExternalInput").ap()
        self.inp[name] = t
        return t

    def dscr(self, name, shape, dt=F32, out=False):
        kind = "ExternalOutput" if (out or self.debug) else "Internal"
        t = self.nc.dram_tensor(name, list(shape), dt, kind=kind).ap()
        if kind == "ExternalOutput":
            self.outs.append(name)
        return t

    def sb(self, es, name, shape, dt=F32):
        self.uid = getattr(self, "uid", 0) + 1
        return es.enter_context(self.nc.sbuf_tensor(f"{name}_u{self.uid}", list(shape), dt))

    def ps(self, es, name, shape, dt=F32):
        self.uid = getattr(self, "uid", 0) + 1
        return es.enter_context(self.nc.psum_tensor(f"{name}_u{self.uid}", list(shape), dt))

    def declare(self):
        L = DEPTH
        d = self.din
        self.x = d("x", [T, D])
        self.c = d("c", [NSEQ, D])
        self.mod_w = d("mod_w", [L, D, 6 * D])
        self.mod_b = d("mod_b", [L, 6 * D])
        self.norm1_g = d("norm1_g", [L, D])
        self.w_in = d("w_in", [L, D, N_IN])
        self.conv_k = d("conv_k", [L, 31, 256])
        self.conv_b = d("conv_b", [L, 256])
        self.conv_ln_g = d("conv_ln_g", [L, 256])
        self.conv_ln_b = d("conv_ln_b", [L, 256])
        self.q_norm_g = d("q_norm_g", [L, 256])
        self.kv_norm_g = d("kv_norm_g", [L, 128])
        self.w_uq = d("w_uq", [L, 256, 512])
        self.w_uk = d("w_uk", [L, 128, 512])
        self.w_uv = d("w_uv", [L, 128, 512])
        self.pool_w = d("pool_w", [L, 4, 64, 64])
        self.pool_scale = d("pool_scale", [L, 256])
        self.w_out = d("w_out", [L, D, D])
        self.norm2_g = d("norm2_g", [L, D])
        self.router_w = d("router_w", [L, D, 36])
        self.router_b = d("router_b", [L, 36])
        self.exp_w1 = [[d(f"exp_w1_{a}_{h}", [NEXP * 128, 2048]) for h in range(2)] for a in range(L)]
        self.exp_w3 = [[d(f"exp_w3_{a}_{h}", [NEXP * 128, 2048]) for h in range(2)] for a in range(L)]
        self.exp_w2 = [[d(f"exp_w2_{a}_{h}", [NEXP * 128, 2048]) for h in range(2)] for a in range(L)]
        self.final_g = d("final_g", [1, D])
        self.cf_d = d("cf", [128, 1024])
        self.cb_d = d("cb", [128, 512], BF16)
        self.pinv_d = d("pinv", [128, 2, SEQ])
        self.out = self.nc.dram_tensor("out", [T, D], F32, kind="ExternalOutput").ap()
        s = self.dscr
        self.xres = s("xres", [T, D])
        self.zA = s("zA", [NSEQ, 512, SEQ])
        self.zCQ = s("zCQ", [NSEQ, 256, SEQ])
        self.zCKV = s("zCKV", [NSEQ, 128, SEQ])
        self.zQI = s("zQI", [NSEQ, 512, SEQ], BF16)
        self.zKI = s("zKI", [NSEQ, 64, SEQ], BF16)
        self.zWI = s("zWI", [NSEQ, SEQ, 8])
        self.zUP = s("zUP", [NSEQ, 256, SEQ])
        self.mixT = s("mixT", [NSEQ, D, SEQ], BF16)
        self.bcD = s("bcD", [128, NSEQ, 6, D])
        self.H2 = s("H2", [T, D], BF16)
        self.Hs = s("Hs", [PSLOT, D], BF16)
        self.Ys = s("Ys", [PSLOT, D])

    def setup(self):
        S, es = self.S, self.es
        self.cf = self.sb(es, "cf_sb", [128, 1024])
        self.cb = self.sb(es, "cb_sb", [128, 512], BF16)
        self.condT = self.sb(es, "condT", [128, 8, NSEQ])
        self.EALL = self.sb(es, "EALL", [128, NT, 64])
        self.RK = self.sb(es, "RK", [128, NT, 2])
        self.GT = self.sb(es, "GT", [128, NT, 2])
        self.DSTI = self.sb(es, "DSTI", [128, NT, 2], I32)
        self.BEXP = self.sb(es, "BEXP", [128, NBLK], I32)
        S.dma('sp', lambda e: e.dma_start(out=self.cf[:], in_=self.cf_d[:, :]), w=[self.cf])
        S.dma('sp', lambda e: e.dma_start(out=self.cb[:], in_=self.cb_d[:, :]), w=[self.cb])
        with self.nc.allow_non_contiguous_dma(reason="tiny transposed load of c"):
            for b in range(NSEQ):
                S.dma('sp', lambda e: e.dma_start(
                    out=self.condT[:, :, b:b + 1],
                    in_=self.c[b:b + 1, :].rearrange("b (k p) -> p k b", p=128)), w=[self.condT])
        S.op('act', lambda e: e.activation(out=self.condT[:], in_=self.condT[:], func=AF.Silu),
             r=[self.condT], w=[self.condT])
        self.ident_f = self.cf[:, 0:128]
        self.ones_f = self.cf[:, 128:256]
        self.ident_b = self.cb[:, 0:128]
        self.ones_b = self.cb[:, 128:256]
        self.triu_b = self.cb[:, 256:384]

    def phase_mod(self, l):
        S, nc = self.S, self.nc
        with ExitStack() as es:
            modrow = self.sb(es, "modrow", [2, 6 * D])
            bc = self.sb(es, "bc_sb", [128, NSEQ, 6, D])
            wbuf = [self.sb(es, f"modw{i}", [128, 8, 512]) for i in range(2)]
            bbuf = [self.sb(es, f"modb{i}", [1, 512]) for i in range(2)]
            ng = [self.sb(es, f"ng{i}", [128, D]) for i in range(2)]
            psm = [self.ps(es, f"psm{i}", [128, 512]) for i in range(2)]
            psb = [self.ps(es, f"psb{i}", [128, 512]) for i in range(2)]
            S.dma('sp', lambda e: e.dma_start(out=ng[0][:], in_=self.norm1_g[l:l + 1, :].partition_broadcast(128)), w=[ng[0]])
            S.dma('sp', lambda e: e.dma_start(out=ng[1][:], in_=self.norm2_g[l:l + 1, :].partition_broadcast(128)), w=[ng[1]])
            mw = self.mod_w[l].rearrange("(k p) n -> p k n", p=128)
            for j in range(12):
                wb, bb, pm = wbuf[j % 2], bbuf[j % 2], psm[j % 2]
                S.dma('sp', lambda e: e.dma_start(out=wb[:], in_=mw[:, :, j * 512:(j + 1) * 512]), w=[wb])
                S.dma('sp', lambda e: e.dma_start(out=bb[:], in_=self.mod_b[l:l + 1, j * 512:(j + 1) * 512]), w=[bb])
                for k in range(8):
                    S.op('pe', lambda e: e.matmul(pm[0:2, :], lhsT=self.condT[:, k, :], rhs=wb[:, k, :],
                                                  start=(k == 0), stop=False),
                         r=[self.condT, wb], w=[pm])
                S.op('pe', lambda e: e.matmul(pm[0:2, :], lhsT=self.ones_f[0:1, 0:2], rhs=bb[0:1, :],
                                              start=False, stop=True), r=[self.cf, bb], w=[pm])
                S.op('act', lambda e: e.copy(out=modrow[0:2, j * 512:(j + 1) * 512], in_=pm[0:2, :]),
                     r=[pm], w=[modrow])
            n = 0
            for b in range(NSEQ):
                sel = self.cf[0:2, 640 + 128 * b:768 + 128 * b]
                for kind in range(6):
                    for half in range(2):
                        pb = psb[n % 2]
                        n += 1
                        c0 = kind * D + half * 512
                        S.op('pe', lambda e: e.matmul(pb[:], lhsT=sel, rhs=modrow[0:2, c0:c0 + 512],
                                                      start=True, stop=True), r=[self.cf, modrow], w=[pb])
                        dst = bc[:, b, kind, half * 512:(half + 1) * 512]
                        if kind in (1, 4):
                            g = ng[0 if kind == 1 else 1]
                            S.op('dve', lambda e: e.scalar_tensor_tensor(
                                out=dst, in0=pb[:], scalar=1.0, in1=g[:, half * 512:(half + 1) * 512],
                                op0=ALU.add, op1=ALU.mult), r=[pb, g], w=[bc])
                        else:
                            S.op('act', lambda e: e.copy(out=dst, in_=pb[:]), r=[pb], w=[bc])
            for b in range(NSEQ):
                S.dma('sp', lambda e: e.dma_start(out=self.bcD[:, b, :, :], in_=bc[:, b, :, :]), r=[bc], w=[('bcD', b)])
        S.barrier()

    def rms_rstd(self, xin, junk, ss, rstd, n):
        S = self.S
        S.op('act', lambda e: e.activation(out=junk[:], in_=xin[:], func=AF.Square, accum_out=ss[:, 0:1]),
             r=[xin], w=[junk, ss])
        S.op('dve', lambda e: e.tensor_scalar(out=ss[:, 0:1], in0=ss[:, 0:1], scalar1=1.0 / n, scalar2=EPS,
                                              op0=ALU.mult, op1=ALU.add), r=[ss], w=[ss])
        S.op('act', lambda e: e.sqrt(out=ss[:, 0:1], in_=ss[:, 0:1]), r=[ss], w=[ss])
        S.op('dve', lambda e: e.reciprocal(out=rstd[:, 0:1], in_=ss[:, 0:1]), r=[ss], w=[rstd])

    def phase_front(self, l):
        S, nc = self.S, self.nc
        xsrc = self.x if l == 0 else self.xres
        chunks = [(0, 128), (128, 128), (256, 128), (384, 128), (512, 128), (640, 128), (768, 128),
                  (896, 128), (1024, 128), (1152, 128), (1280, 128), (1408, 64), (1472, 8),
                  (1480, 128), (1608, 128)]
        NG = NT // 4
        with ExitStack() as es:
            win = self.sb(es, "win", [128, 8, N_IN], BF16)
            bcl = self.sb(es, "bcl", [128, NSEQ, 2, D])
            for b_ in range(NSEQ):
                S.dma('sp', lambda e: e.dma_start(out=bcl[:, b_, :, :], in_=self.bcD[:, b_, 0:2, :]), w=[bcl])
            xin = [self.sb(es, f"xin{i}", [128, D]) for i in range(4)]
            junk = self.sb(es, "junk", [128, D], BF16)
            hf = [self.sb(es, f"hf{i}", [128, D]) for i in range(2)]
            hb = [self.sb(es, f"hb{i}", [128, D], BF16) for i in range(4)]
            hT = [self.sb(es, f"hT{i}", [128, 8, 512], BF16) for i in range(2)]
            ss = [self.sb(es, f"ss{i}", [128, 4]) for i in range(2)]
            zf = [self.sb(es, f"zf{i}", [128, 512]) for i in range(10)]
            zb = [self.sb(es, f"zb{i}", [128, 512], BF16) for i in range(5)]
            wiT = self.sb(es, "wiT", [8, 512])
            wi_tm = self.sb(es, "wi_tm", [128, 4, 8])
            tp = [self.ps(es, f"tp{i}", [128, D], BF16) for i in range(2)]
            pz = [self.ps(es, f"pz{i}", [128, 512]) for i in range(4)]
            pw = self.ps(es, "pw", [128, 4, 8])
            S.dma('pool', lambda e: e.dma_start(out=win[:], in_=self.w_in[l].rearrange("(k p) n -> p k n", p=128)),
                  w=[win])
            cnt = {"zf": 0, "zb": 0, "pz": 0, "tp": 0}

            def loads(g):
                for t in range(4):
                    i = g * 4 + t
                    S.dma('sp', lambda e: e.dma_start(out=xin[t][:], in_=xsrc[i * 128:(i + 1) * 128, :]), w=[xin[t]])

            def norm(g):
                b = g // (NG // NSEQ)
                s_ = ss[g % 2]
                for t in range(4):
                    S.op('act', lambda e: e.activation(out=junk[:], in_=xin[t][:], func=AF.Square,
                                                       accum_out=s_[:, t:t + 1]), r=[xin[t]], w=[junk, (s_.name, t)])
                keys = [(s_.name, t) for t in range(4)]
                S.op('dve', lambda e: e.tensor_scalar(out=s_[:], in0=s_[:], scalar1=1.0 / D, scalar2=EPS,
                                                      op0=ALU.mult, op1=ALU.add), r=keys, w=keys)
                S.op('act', lambda e: e.sqrt(out=s_[:], in_=s_[:]), r=keys, w=keys)
                S.op('dve', lambda e: e.reciprocal(out=s_[:], in_=s_[:]), r=keys, w=keys)
                for t in range(4):
                    h_ = hf[t % 2]
                    S.op('dve', lambda e: e.scalar_tensor_tensor(out=h_[:], in0=xin[t][:], scalar=s_[:, t:t + 1],
                                                                 in1=bcl[:, b, 1, :], op0=ALU.mult, op1=ALU.mult),
                         r=[xin[t], (s_.name, t), bcl], w=[h_])
                    S.op('pool', lambda e: e.tensor_tensor(out=hb[t][:], in0=h_[:], in1=bcl[:, b, 0, :], op=ALU.add),
                         r=[h_, bcl], w=[hb[t]])

            def trans(g):
                hTg = hT[g % 2]
                for t in range(4):
                    tpi = tp[cnt["tp"] % 2]
                    cnt["tp"] += 1
                    for k in range(8):
                        S.op('pe', lambda e: e.transpose(tpi[:, k * 128:(k + 1) * 128], hb[t][:, k * 128:(k + 1) * 128],
                                                         self.ident_b), r=[hb[t], self.cb], w=[tpi])
                    S.op('act', lambda e: e.copy(out=hTg[:, :, t * 128:(t + 1) * 128],
                                                 in_=tpi[:].rearrange("p (k t) -> p k t", k=8)), r=[tpi],
                         w=[(hTg.name, t)])

            def proj(g):
                stores = []
                hTg = hT[g % 2]
                hkeys = [(hTg.name, t) for t in range(4)]
                b = g // (NG // NSEQ)
                gs = g % (NG // NSEQ)
                cs = slice(gs * 512, (gs + 1) * 512)
                for ci, (c0, m) in enumerate(chunks):
                    p = pz[cnt["pz"] % 4]
                    cnt["pz"] += 1
                    for k in range(8):
                        S.op('pe', lambda e: e.matmul(p[0:m, :], lhsT=win[:, k, c0:c0 + m], rhs=hTg[:, k, :],
                                                      start=(k == 0), stop=(k == 7)), r=[win] + hkeys, w=[p])
                    if 7 <= ci <= 11:
                        z = zb[cnt["zb"] % 5]
                        cnt["zb"] += 1
                        S.op('act', lambda e: e.copy(out=z[0:m, :], in_=p[0:m, :]), r=[p], w=[z])
                        dst = self.zQI[b, (ci - 7) * 128:(ci - 6) * 128, cs] if ci < 11 else self.zKI[b, :, cs]
                        stores.append(lambda dst=dst, z=z, m=m, ci=ci: S.dma(
                            'sp', lambda e: e.dma_start(out=dst, in_=z[0:m, :]), r=[z], w=[("z", ci, g)]))
                    elif ci == 12:
                        S.op('act', lambda e: e.copy(out=wiT[:], in_=p[0:8, :]), r=[p], w=[wiT])
                        for s4 in range(4):
                            S.op('pe', lambda e: e.transpose(pw[:, s4, :], wiT[0:8, s4 * 128:(s4 + 1) * 128],
                                                             self.ident_f[0:8, 0:8]), r=[wiT, self.cf], w=[pw])
                        S.op('act', lambda e: e.copy(out=wi_tm[:], in_=pw[:]), r=[pw], w=[wi_tm])
                        stores.append(lambda: S.dma('sp', lambda e: e.dma_start(
                            out=self.zWI[b, cs, :].rearrange("(s p) h -> p s h", p=128), in_=wi_tm[:]),
                            r=[wi_tm], w=[("z", 12, g)]))
                    else:
                        z = zf[cnt["zf"] % 10]
                        cnt["zf"] += 1
                        S.op('dve', lambda e: e.tensor_copy(out=z[0:m, :], in_=p[0:m, :]), r=[p], w=[z])
                        if ci < 4:
                            dst = self.zA[b, ci * 128:(ci + 1) * 128, cs]
                        elif ci < 6:
                            dst = self.zCQ[b, (ci - 4) * 128:(ci - 3) * 128, cs]
                        elif ci == 6:
                            dst = self.zCKV[b, :, cs]
                        else:
                            dst = self.zUP[b, (ci - 13) * 128:(ci - 12) * 128, cs]
                        stores.append(lambda dst=dst, z=z, m=m, ci=ci: S.dma(
                            'sp', lambda e: e.dma_start(out=dst, in_=z[0:m, :]), r=[z], w=[("z", ci, g)]))
                return stores

            loads(0)
            norm(0)
            trans(0)
            pending = []
            for g in range(NG):
                if g + 1 < NG:
                    loads(g + 1)
                for fn in pending:
                    fn()
                if g + 1 < NG:
                    norm(g + 1)
                pending = proj(g)
                if g + 1 < NG:
                    trans(g + 1)
            for fn in pending:
                fn()
        S.barrier()

    def phase_conv(self, l):
        S, nc = self.S, self.nc
        with ExitStack() as es:
            ck = self.sb(es, "ck", [128, 2, 31])
            cv = self.sb(es, "cv", [128, 3, 2])
            dg = self.sb(es, "cdg", [128, 2, 31, 128], BF16)
            av = [self.sb(es, f"av{i}", [128, SEQ]) for i in range(2)]
            ag = [self.sb(es, f"ag{i}", [128, SEQ]) for i in range(2)]
            upad = [self.sb(es, f"upad{i}", [128, 30 + SEQ], BF16) for i in range(2)]
            acc = [self.sb(es, f"cacc{i}", [128, 512]) for i in range(2)]
            sq = [self.sb(es, f"csq{i}", [128, 512]) for i in range(2)]
            mean = self.sb(es, "cmean", [128, 512])
            var = self.sb(es, "cvar", [128, 512])
            t1 = [self.sb(es, f"ct1{i}", [128, 512]) for i in range(2)]
            yb = [self.sb(es, f"cyb{i}", [128, 512], BF16) for i in range(4)]
            pcv = [self.ps(es, f"cpc{i}", [128, 512]) for i in range(4)]
            pss = self.ps(es, "cps_s", [128, 512])
            psq = self.ps(es, "cps_q", [128, 512])
            with nc.allow_non_contiguous_dma(reason="tiny transposed parameter loads"):
                for ch in range(2):
                    S.dma('sp', lambda e: e.dma_start(
                        out=ck[:, ch, :], in_=self.conv_k[l][:, ch * 128:(ch + 1) * 128].rearrange("k p -> p k")), w=[ck])
                for j, src in enumerate((self.conv_b, self.conv_ln_g, self.conv_ln_b)):
                    S.dma('sp', lambda e: e.dma_start(out=cv[:, j, :], in_=src[l].rearrange("(c p) -> p c", p=128)),
                          w=[cv])
            for ch in range(2):
                S.op('pool', lambda e: e.tensor_tensor(
                    out=dg[:, ch, :, :], in0=self.ident_b.unsqueeze(1).to_broadcast([128, 31, 128]),
                    in1=ck[:, ch, :].unsqueeze(2).to_broadcast([128, 31, 128]), op=ALU.mult),
                    r=[self.cb, ck], w=[dg])
            for i in range(2):
                S.op('pool', lambda e: e.memset(upad[i][:, 0:30], 0.0), w=[upad[i]])
            cepsb = self.sb(es, "ceps", [128, 1])
            S.op('pool', lambda e: e.memset(cepsb[:], EPS), w=[cepsb])
            ny = 0
            npc = 0
            for b in range(NSEQ):
                for ch in range(2):
                    S.dma('sp', lambda e: e.dma_start(out=av[ch][:], in_=self.zA[b, ch * 128:(ch + 1) * 128, :]),
                          w=[av[ch]])
                    S.dma('sp', lambda e: e.dma_start(out=ag[ch][:],
                                                      in_=self.zA[b, 256 + ch * 128:256 + (ch + 1) * 128, :]),
                          w=[ag[ch]])
                    S.op('act', lambda e: e.activation(out=ag[ch][:], in_=ag[ch][:], func=AF.Sigmoid), r=[ag[ch]],
                         w=[ag[ch]])
                    S.op('dve' if ch == 0 else 'pool',
                         lambda e: e.tensor_tensor(out=upad[ch][:, 30:], in0=av[ch][:], in1=ag[ch][:], op=ALU.mult),
                         r=[av[ch], ag[ch]], w=[upad[ch]])
                for tt in range(4):
                    cs = slice(tt * 512, (tt + 1) * 512)
                    for ch in range(2):
                        p = pcv[npc % 4]
                        npc += 1
                        for k in range(31):
                            S.op('pe', lambda e: e.matmul(p[:], lhsT=dg[:, ch, k, :],
                                                          rhs=upad[ch][:, tt * 512 + k:tt * 512 + k + 512],
                                                          start=(k == 0), stop=(k == 30)), r=[dg, upad[ch]], w=[p])
                        S.op('act', lambda e: e.activation(out=acc[ch][:], in_=p[:], func=AF.Identity,
                                                           bias=cv[:, 0, ch:ch + 1], scale=1.0), r=[p, cv], w=[acc[ch]])
                        S.op('act', lambda e: e.activation(out=sq[ch][:], in_=acc[ch][:], func=AF.Square),
                             r=[acc[ch]], w=[sq[ch]])
                    for ch in range(2):
                        S.op('pe', lambda e: e.matmul(pss[:], lhsT=self.ones_f, rhs=acc[ch][:], start=(ch == 0),
                                                      stop=(ch == 1)), r=[acc[ch], self.cf], w=[pss])
                    for ch in range(2):
                        S.op('pe', lambda e: e.matmul(psq[:], lhsT=self.ones_f, rhs=sq[ch][:], start=(ch == 0),
                                                      stop=(ch == 1)), r=[sq[ch], self.cf], w=[psq])
                    S.op('act', lambda e: e.mul(out=mean[:], in_=pss[:], mul=1.0 / 256), r=[pss], w=[mean])
                    S.op('dve', lambda e: e.tensor_tensor(out=var[:], in0=mean[:], in1=mean[:], op=ALU.mult),
                         r=[mean], w=[var])
                    S.op('dve', lambda e: e.scalar_tensor_tensor(out=var[:], in0=psq[:], scalar=1.0 / 256, in1=var[:],
                                                                 op0=ALU.mult, op1=ALU.subtract), r=[psq, var], w=[var])
                    S.op('act', lambda e: e.activation(out=var[:], in_=var[:], func=AF.Ln, bias=cepsb[:, 0:1], scale=1.0),
                         r=[var, cepsb], w=[var])
                    S.op('act', lambda e: e.activation(out=var[:], in_=var[:], func=AF.Exp, scale=-0.5), r=[var], w=[var])
                    for ch in range(2):
                        y = yb[ny % 4]
                        ny += 1
                        S.op('dve', lambda e: e.tensor_tensor(out=t1[ch][:], in0=acc[ch][:], in1=mean[:],
                                                              op=ALU.subtract), r=[acc[ch], mean], w=[t1[ch]])
                        S.op('dve', lambda e: e.tensor_tensor(out=t1[ch][:], in0=t1[ch][:], in1=var[:], op=ALU.mult),
                             r=[t1[ch], var], w=[t1[ch]])
                        S.op('act', lambda e: e.activation(out=y[:], in_=t1[ch][:], func=AF.Silu,
                                                           bias=cv[:, 2, ch:ch + 1], scale=cv[:, 1, ch:ch + 1]),
                             r=[t1[ch], cv], w=[y])
                        S.dma('pool', lambda e: e.dma_start(out=self.mixT[b, ch * 128:(ch + 1) * 128, cs], in_=y[:]),
                              r=[y], w=[("mixT", b, ch, tt)])
        S.barrier()

    def phase_pool(self, l):
        S, nc = self.S, self.nc
        with ExitStack() as es:
            pinv = self.sb(es, "pinv_sb", [128, 2, SEQ])
            bdf = self.sb(es, "bdf", [128, 2, 128])
            bd = self.sb(es, "bd", [128, 2, 128], BF16)
            psc = self.sb(es, "psc", [128, 2])
            P = [self.sb(es, f"pp{i}", [128, 16 + SEQ]) for i in range(5)]
            dd = self.sb(es, "pd", [128, SEQ])
            db = self.sb(es, "pdb", [128, SEQ], BF16)
            yb = [self.sb(es, f"pyb{i}", [128, 512], BF16) for i in range(2)]
            pp = [self.ps(es, f"pps{i}", [128, 512]) for i in range(2)]
            S.dma('sp', lambda e: e.dma_start(out=pinv[:], in_=self.pinv_d[:, :, :]), w=[pinv])
            S.op('pool', lambda e: e.memset(bdf[:], 0.0), w=[bdf])
            for g in range(4):
                ch, half = divmod(g, 2)
                S.dma('sp', lambda e: e.dma_start(out=bdf[half * 64:(half + 1) * 64, ch, half * 64:(half + 1) * 64],
                                                  in_=self.pool_w[l, g]), w=[bdf])
            S.op('act', lambda e: e.copy(out=bd[:], in_=bdf[:]), r=[bdf], w=[bd])
            with nc.allow_non_contiguous_dma(reason="tiny transposed parameter load"):
                S.dma('sp', lambda e: e.dma_start(out=psc[:], in_=self.pool_scale[l].rearrange("(c p) -> p c", p=128)),
                      w=[psc])
            for i in range(5):
                S.op('pool', lambda e: e.memset(P[i][:, 0:16], 0.0), w=[P[i]])
            n = 0
            for b in range(NSEQ):
                for ch in range(2):
                    S.dma('sp', lambda e: e.dma_start(out=P[0][:, 16:], in_=self.zUP[b, ch * 128:(ch + 1) * 128, :]),
                          w=[P[0]])
                    for j, sh in enumerate((1, 2, 4, 8)):
                        if ch == 0 and j >= 2:
                            break
                        eng = 'dve'
                        S.op(eng, lambda e: e.tensor_tensor(out=P[j + 1][:, 16:], in0=P[j][:, 16:],
                                                            in1=P[j][:, 16 - sh:16 - sh + SEQ], op=ALU.add),
                             r=[P[j]], w=[P[j + 1]])
                    lo_src, hi_src = (P[1], P[2]) if ch == 0 else (P[3], P[4])
                    for half, src in ((0, lo_src), (1, hi_src)):
                        ps_ = slice(half * 64, (half + 1) * 64)
                        S.op('dve', lambda e: e.tensor_tensor(out=dd[ps_, :], in0=src[ps_, 16:], in1=pinv[ps_, ch, :],
                                                              op=ALU.mult), r=[src, pinv], w=[dd])
                    S.op('dve', lambda e: e.tensor_tensor(out=db[:], in0=dd[:], in1=P[0][:, 16:], op=ALU.subtract),
                         r=[dd, P[0]], w=[db])
                    for tt in range(4):
                        cs = slice(tt * 512, (tt + 1) * 512)
                        p = pp[n % 2]
                        y = yb[n % 2]
                        n += 1
                        S.op('pe', lambda e: e.matmul(p[:], lhsT=bd[:, ch, :], rhs=db[:, cs], start=True, stop=True),
                             r=[bd, db], w=[p])
                        S.op('act', lambda e: e.mul(out=y[:], in_=p[:], mul=psc[:, ch:ch + 1]), r=[p, psc], w=[y])
                        S.dma('sp', lambda e: e.dma_start(out=self.mixT[b, 768 + ch * 128:768 + (ch + 1) * 128, cs],
                                                          in_=y[:]), r=[y], w=[("mixT", b, 6 + ch, tt)])
        S.barrier()

    def phase_attn(self, l, nit=12):
        S, nc = self.S, self.nc
        NQB = SEQ // 128
        with ExitStack() as es:
            pb = [self.ps(es, f"ab{i}", [128, 512]) for i in range(8)]
            wuq = self.sb(es, "wuq", [128, 2, 512], BF16)
            wukf = self.sb(es, "wukf", [128, 512])
            wukT = self.sb(es, "wukT", [128, 4, 128], BF16)
            wuvf = self.sb(es, "wuvf", [128, 8, 128])
            wuvp = self.sb(es, "wuvp", [128, 8, 128], BF16)
            gq = self.sb(es, "gq", [128, 2])
            gkv = self.sb(es, "gkv", [128, 1])
            cqn = self.sb(es, "cqn", [128, 2, SEQ], BF16)
            ckvT = self.sb(es, "ckvT", [128, SEQ], BF16)
            vtok = self.sb(es, "vtok", [128, NQB, 128], BF16)
            qabs = self.sb(es, "qabs", [128, 8, SEQ], BF16)
            qiT = self.sb(es, "qiT", [128, 4, SEQ], BF16)
            ki2 = self.sb(es, "ki2", [128, SEQ], BF16)
            wi = self.sb(es, "wi", [128, NQB, 8])
            wab = self.sb(es, "wab", [128, NQB, 8])
            wsg = self.sb(es, "wsg", [128, NQB, 8])
            ctxn = self.sb(es, "ctxn", [128, 8, SEQ], BF16)
            qT = ctxn
            ld = [self.sb(es, f"ald{i}", [128, SEQ]) for i in range(2)]
            sqs = [self.sb(es, f"asq{i}", [128, 512]) for i in range(4)]
            rss = [self.sb(es, f"ars{i}", [128, 512]) for i in range(2)]
            dsg = self.sb(es, "dsg", [128, 8, 128], BF16)
            term = [self.sb(es, f"term{i}", [128, 512], BF16) for i in range(3)]
            scs = ld
            junk = self.sb(es, "ajunk", [128, SEQ], BF16)
            masks = [self.sb(es, f"mask{i}", [128, SEQ], BF16) for i in range(2)]
            maskT = [self.sb(es, f"maskT{i}", [128, NQB, 128], BF16) for i in range(2)]
            PT = [self.sb(es, f"PT{i}", [128, 512], BF16) for i in range(4)]
            PTm = [self.sb(es, f"PTm{i}", [128, 512], BF16) for i in range(2)]
            rden = self.sb(es, "rden", [128, 1024])
            st = self.sb(es, "ast", [128, 8])
            nbias = self.sb(es, "anb", [128, 1])
            epsb = self.sb(es, "aeps", [128, 1])
            S.op('pool', lambda e: e.memset(epsb[:], EPS), w=[epsb])
            S.op('pool', lambda e: e.memset(nbias[:], -30000.0), w=[nbias])
            W = self.sb(es, "aW", [128, nit + 1])
            W2 = self.sb(es, "aW2", [128, nit + 1])
            ob = [self.sb(es, f"aob{i}", [128, 512], BF16) for i in range(2)]
            S.dma('pool', lambda e: e.dma_start(out=wuq[:], in_=self.w_uq[l].rearrange("(k p) n -> p k n", p=128)),
                  w=[wuq])
            S.dma('sp', lambda e: e.dma_start(out=wukf[:], in_=self.w_uk[l]), w=[wukf])
            pv = pb[0][:].rearrange("p (j k) -> p j k", j=4)
            for j in range(4):
                S.op('pe', lambda e: e.transpose(pv[:, j, :], wukf[:, j * 128:(j + 1) * 128], self.ident_f),
                     r=[wukf, self.cf], w=[pb[0]])
            S.op('act', lambda e: e.copy(out=wukT[:], in_=pv), r=[pb[0]], w=[wukT])
            S.op('pool', lambda e: e.memset(wuvf[:], 0.0), w=[wuvf])
            wv = self.w_uv[l].rearrange("k (j two d) -> k j two d", two=2, d=64)
            wvf = wuvf[:].rearrange("p (j two) c -> p j two c", two=2)
            for par in range(2):
                S.dma('sp', lambda e: e.dma_start(out=wvf[:, :, par, par * 64:(par + 1) * 64], in_=wv[:, :, par, :]),
                      w=[wuvf])
            S.op('act', lambda e: e.copy(out=wuvp[:], in_=wuvf[:]), r=[wuvf], w=[wuvp])
            with nc.allow_non_contiguous_dma(reason="tiny transposed parameter loads"):
                S.dma('sp', lambda e: e.dma_start(out=gq[:], in_=self.q_norm_g[l].rearrange("(c p) -> p c", p=128)),
                      w=[gq])
                S.dma('sp', lambda e: e.dma_start(out=gkv[:], in_=self.kv_norm_g[l].rearrange("(c p) -> p c", p=128)),
                      w=[gkv])
            for b in range(NSEQ):
                for ch in range(2):
                    S.dma('sp', lambda e: e.dma_start(out=ld[ch][:], in_=self.zCQ[b, ch * 128:(ch + 1) * 128, :]),
                          w=[ld[ch]])
                for tt in range(4):
                    cs = slice(tt * 512, (tt + 1) * 512)
                    rs = rss[tt % 2]
                    pn = pb[tt % 2]
                    for ch in range(2):
                        sq = sqs[(tt % 2) * 2 + ch]
                        S.op('act', lambda e: e.activation(out=sq[:], in_=ld[ch][:, cs], func=AF.Square),
                             r=[ld[ch]], w=[sq])
                        S.op('pe', lambda e: e.matmul(pn[:], lhsT=self.ones_f, rhs=sq[:], start=(ch == 0),
                                                      stop=(ch == 1)), r=[sq, self.cf], w=[pn])
                    S.op('act', lambda e: e.activation(out=rs[:], in_=pn[:], func=AF.Ln, bias=epsb[:, 0:1],
                                                       scale=1.0 / 256), r=[pn, epsb], w=[rs])
                    S.op('act', lambda e: e.activation(out=rs[:], in_=rs[:], func=AF.Exp, scale=-0.5), r=[rs], w=[rs])
                    for ch in range(2):
                        S.op('dve', lambda e: e.scalar_tensor_tensor(out=cqn[:, ch, cs], in0=ld[ch][:, cs],
                                                                     scalar=gq[:, ch:ch + 1], in1=rs[:],
                                                                     op0=ALU.mult, op1=ALU.mult),
                             r=[ld[ch], gq, rs], w=[cqn])
                S.dma('sp', lambda e: e.dma_start(out=ld[0][:], in_=self.zCKV[b, :, :]), w=[ld[0]])
                for tt in range(4):
                    cs = slice(tt * 512, (tt + 1) * 512)
                    rs = rss[tt % 2]
                    sq = sqs[tt % 4]
                    pn = pb[tt % 2]
                    S.op('act', lambda e: e.activation(out=sq[:], in_=ld[0][:, cs], func=AF.Square), r=[ld[0]], w=[sq])
                    S.op('pe', lambda e: e.matmul(pn[:], lhsT=self.ones_f, rhs=sq[:], start=True, stop=True),
                         r=[sq, self.cf], w=[pn])
                    S.op('act', lambda e: e.activation(out=rs[:], in_=pn[:], func=AF.Ln, bias=epsb[:, 0:1],
                                                       scale=1.0 / 128), r=[pn, epsb], w=[rs])
                    S.op('act', lambda e: e.activation(out=rs[:], in_=rs[:], func=AF.Exp, scale=-0.5), r=[rs], w=[rs])
                    S.op('dve', lambda e: e.scalar_tensor_tensor(out=ckvT[:, cs], in0=ld[0][:, cs], scalar=gkv[:, 0:1],
                                                                 in1=rs[:], op0=ALU.mult, op1=ALU.mult),
                         r=[ld[0], gkv, rs], w=[ckvT])
                for half in range(2):
                    tpv = pb[2 + half][:].bitcast(BF16).rearrange("p (j k) -> p j k", j=8)
                    for j in range(8):
                        sci = half * 8 + j
                        S.op('pe', lambda e: e.transpose(tpv[:, j, :], ckvT[:, sci * 128:(sci + 1) * 128], self.ident_b),
                             r=[ckvT, self.cb], w=[pb[2 + half]])
                    S.op('act', lambda e: e.copy(out=vtok[:, half * 8:(half + 1) * 8, :], in_=tpv),
                         r=[pb[2 + half]], w=[vtok])
                n = 0
                for m in range(4):
                    for tt in range(4):
                        cs = slice(tt * 512, (tt + 1) * 512)
                        p = pb[4 + n % 2]
                        n += 1
                        for k in range(2):
                            S.op('pe', lambda e: e.matmul(p[:], lhsT=wuq[:, k, m * 128:(m + 1) * 128], rhs=cqn[:, k, cs],
                                                          start=(k == 0), stop=(k == 1)), r=[wuq, cqn], w=[p])
                        if n % 2 == 0:
                            S.op('act', lambda e: e.copy(out=qT[:, m, cs], in_=p[:]), r=[p], w=[qT])
                        else:
                            S.op('dve', lambda e: e.tensor_copy(out=qT[:, m, cs], in_=p[:]), r=[p], w=[qT])
                for h in range(8):
                    hp = slice((h % 2) * 64, (h % 2) * 64 + 64)
                    for tt in range(4):
                        cs = slice(tt * 512, (tt + 1) * 512)
                        p = pb[4 + n % 2]
                        n += 1
                        S.op('pe', lambda e: e.matmul(p[:], lhsT=wukT[hp, h // 2, :], rhs=qT[hp, h // 2, cs],
                                                      start=True, stop=True), r=[wukT, qT], w=[p])
                        if n % 2 == 0:
                            S.op('act', lambda e: e.mul(out=qabs[:, h, cs], in_=p[:], mul=0.125), r=[p], w=[qabs])
                        else:
                            S.op('dve', lambda e: e.tensor_scalar(out=qabs[:, h, cs], in0=p[:], scalar1=0.125,
                                                                  scalar2=None, op0=ALU.mult), r=[p], w=[qabs])
                S.dma('sp', lambda e: e.dma_start(out=qiT[:], in_=self.zQI[b].rearrange("(m p) t -> p m t", p=128)),
                      w=[qiT])
                for half in range(2):
                    S.dma('sp', lambda e: e.dma_start(out=ki2[half * 64:(half + 1) * 64, :], in_=self.zKI[b, :, :]),
                          w=[ki2])
                S.dma('sp', lambda e: e.dma_start(out=wi[:], in_=self.zWI[b].rearrange("(q p) h -> p q h", p=128)),
                      w=[wi])
                S.op('act', lambda e: e.activation(out=wab[:], in_=wi[:], func=AF.Abs, scale=float(512 ** -0.5)),
                     r=[wi], w=[wab])
                S.op('act', lambda e: e.activation(out=wsg[:], in_=wi[:], func=AF.Sign), r=[wi], w=[wsg])
                def indexer(qb):
                    sc = scs[qb % 2]
                    nk = (qb + 1) * 128
                    nkt = (nk + 511) // 512
                    qs = slice(qb * 128, (qb + 1) * 128)
                    S.op('pool', lambda e: e.tensor_tensor(
                        out=dsg[:], in0=self.ident_b.unsqueeze(1).to_broadcast([128, 8, 128]),
                        in1=wsg[:, qb, :].unsqueeze(2).to_broadcast([128, 8, 128]), op=ALU.mult),
                        r=[self.cb, wsg], w=[dsg])
                    for kt in range(nkt):
                        w_ = min(512, nk - kt * 512)
                        ks = slice(kt * 512, kt * 512 + w_)
                        psc = pb[1]

                        def rel(h):
                            hp = slice((h % 2) * 64, (h % 2) * 64 + 64)
                            p = pb[0]
                            S.op('pe', lambda e: e.matmul(p[:, 0:w_], lhsT=qiT[hp, h // 2, qs], rhs=ki2[hp, ks],
                                                          start=True, stop=True), r=[qiT, ki2], w=[p])
                        rel(0)
                        for h in range(8):
                            p = pb[0]
                            tm = term[h % 3]
                            S.op('act', lambda e: e.activation(out=tm[:, 0:w_], in_=p[:, 0:w_], func=AF.Relu,
                                                               scale=wab[:, qb, h:h + 1]), r=[p, wab], w=[tm])
                            if h + 1 < 8:
                                rel(h + 1)
                            S.op('pe', lambda e: e.matmul(psc[:, 0:w_], lhsT=dsg[:, h, :], rhs=tm[:, 0:w_],
                                                          start=(h == 0), stop=(h == 7)), r=[dsg, tm], w=[psc])
                            if h < 7:
                                yield
                        S.op('act', lambda e: e.copy(out=sc[:, ks], in_=psc[:, 0:w_]), r=[psc], w=[sc])
                        yield
                    S.op('pool', lambda e: e.memset(sc[0:64, nk - 64:nk], -1e30), r=[], w=[sc])

                def bisect(qb):
                    sc = scs[qb % 2]
                    mask = masks[qb % 2]
                    nk = (qb + 1) * 128
                    if qb < 2:
                        S.op('dve', lambda e: e.tensor_scalar(out=mask[:, 0:nk], in0=sc[:, 0:nk], scalar1=-1e29,
                                                              scalar2=None, op0=ALU.is_ge), r=[sc], w=[mask])
                        return
                    S.op('dve', lambda e: e.reduce_max(out=st[:, 0:1], in_=sc[:, 0:nk], axis=AX.X), r=[sc], w=[st])
                    S.op('dve', lambda e: e.tensor_reduce(out=st[:, 2:3], in_=sc[:, 0:nk - 64], axis=AX.X, op=ALU.min),
                         r=[sc], w=[st])
                    S.op('dve', lambda e: e.tensor_tensor(out=st[:, 1:2], in0=st[:, 0:1], in1=st[:, 2:3],
                                                          op=ALU.subtract), r=[st], w=[st])
                    S.op('dve', lambda e: e.tensor_scalar(out=W[:], in0=self.cf[:, 896:896 + nit + 1], scalar1=st[:, 1:2],
                                                          scalar2=None, op0=ALU.mult), r=[st, self.cf], w=[W])
                    S.op('dve', lambda e: e.tensor_scalar(out=W2[:], in0=W[:], scalar1=2.0, scalar2=None, op0=ALU.mult),
                         r=[W], w=[W2])
                    S.op('dve', lambda e: e.tensor_tensor(out=st[:, 3:4], in0=st[:, 2:3], in1=W[:, 0:1], op=ALU.add),
                         r=[st, W], w=[st])
                    for i in range(nit):
                        S.op('dve', lambda e: e.tensor_scalar(out=junk[:, 0:nk], in0=sc[:, 0:nk], scalar1=st[:, 3:4],
                                                              scalar2=0.0, op0=ALU.is_ge, op1=ALU.add,
                                                              accum_out=st[:, 4:5]), r=[sc, st], w=[junk, st])
                        S.op('dve', lambda e: e.tensor_scalar(out=st[:, 5:6], in0=st[:, 4:5], scalar1=float(TOPK),
                                                              scalar2=W2[:, i + 1:i + 2], op0=ALU.is_ge, op1=ALU.mult),
                             r=[st, W2], w=[st])
                        S.op('dve', lambda e: e.scalar_tensor_tensor(out=st[:, 3:4], in0=st[:, 5:6],
                                                                     scalar=W[:, i + 1:i + 2], in1=st[:, 3:4],
                                                                     op0=ALU.subtract, op1=ALU.add), r=[st, W], w=[st])
                    S.op('dve', lambda e: e.tensor_tensor(out=st[:, 3:4], in0=st[:, 3:4], in1=W[:, nit:nit + 1],
                                                          op=ALU.subtract), r=[st, W], w=[st])
                    S.op('dve', lambda e: e.tensor_scalar(out=mask[:, 0:nk], in0=sc[:, 0:nk], scalar1=st[:, 3:4],
                                                          scalar2=None, op0=ALU.is_ge), r=[sc, st], w=[mask])

                def masktr(qb):
                    mT = maskT[qb % 2]
                    mask = masks[qb % 2]
                    for half in range((qb + 8) // 8):
                        tpv = pb[1][:].bitcast(BF16).rearrange("p (j k) -> p j k", j=8)
                        nj = min(8, qb + 1 - half * 8)
                        for j in range(nj):
                            sci = half * 8 + j
                            S.op('pe', lambda e: e.transpose(tpv[:, j, :], mask[:, sci * 128:(sci + 1) * 128],
                                                             self.ident_b), r=[mask, self.cb], w=[pb[1]])
                        S.op('act', lambda e: e.activation(out=mT[:, half * 8:half * 8 + nj, :], in_=tpv[:, 0:nj, :],
                                                           func=AF.Identity, bias=nbias[:, 0:1], scale=30000.0),
                             r=[pb[1], nbias], w=[mT])

                def attend(qb):
                    qs = slice(qb * 128, (qb + 1) * 128)
                    mT = maskT[qb % 2]
                    npt = 0

                    def lgmm(sci, hh):
                        pl = pb[2 + hh]
                        S.op('pe', lambda e: e.matmul(pl[:].rearrange("p (h q) -> p h q", h=4),
                                                      lhsT=ckvT[:, sci * 128:(sci + 1) * 128],
                                                      rhs=qabs[:, hh * 4:(hh + 1) * 4, qs], start=True, stop=False),
                             r=[ckvT, qabs], w=[pl])
                        S.op('pe', lambda e: e.matmul(pl[:].rearrange("p (h q) -> p h q", h=4),
                                                      lhsT=self.ident_b,
                                                      rhs=mT[:, sci, :].unsqueeze(1).to_broadcast([128, 4, 128]),
                                                      start=False, stop=True), r=[self.cb, mT], w=[pl])
                    lgmm(0, 0)
                    lgmm(0, 1)
                    for sci in range(qb + 1):
                        for hh in range(2):
                            pl, pc, pd = pb[2 + hh], pb[4 + hh], pb[6 + hh]
                            pt = PT[npt % 4]
                            npt += 1
                            S.op('act', lambda e: e.activation(out=pt[:, 0:512], in_=pl[:], func=AF.Exp),
                                 r=[pl], w=[pt])
                            if sci + 1 <= qb:
                                lgmm(sci + 1, hh)
                            S.op('pe', lambda e: e.matmul(pc[:], lhsT=vtok[:, sci, :], rhs=pt[:, 0:512],
                                                          start=(sci == 0), stop=(sci == qb)), r=[vtok, pt], w=[pc])
                            S.op('pe', lambda e: e.matmul(pd[:], lhsT=self.ones_b, rhs=pt[:, 0:512],
                                                          start=(sci == 0), stop=(sci == qb)), r=[self.cb, pt], w=[pd])
                            yield
                    for hh in range(2):
                        S.op('act', lambda e: e.activation(out=rden[:, hh * 512:(hh + 1) * 512], in_=pb[6 + hh][:],
                                                           func=AF.Ln), r=[pb[6 + hh]], w=[(rden.name, hh)])
                        S.op('act', lambda e: e.activation(out=rden[:, hh * 512:(hh + 1) * 512],
                                                           in_=rden[:, hh * 512:(hh + 1) * 512], func=AF.Exp, scale=-1.0),
                             r=[(rden.name, hh)], w=[(rden.name, hh)])
                        S.op('dve', lambda e: e.tensor_tensor(
                            out=ctxn[:, hh * 4:(hh + 1) * 4, qs],
                            in0=pb[4 + hh][:].rearrange("p (h q) -> p h q", h=4),
                            in1=rden[:, hh * 512:(hh + 1) * 512].rearrange("p (h q) -> p h q", h=4), op=ALU.mult),
                            r=[pb[4 + hh], (rden.name, hh)], w=[ctxn])

                def drain(g):
                    for _ in g:
                        pass

                def interleave(ga, gb):
                    ga, gb = iter(ga), iter(gb)
                    da = db = False
                    while not (da and db):
                        if not da:
                            try:
                                next(ga)
                            except StopIteration:
                                da = True
                        if not db:
                            try:
                                next(gb)
                            except StopIteration:
                                db = True

                drain(indexer(0))
                bisect(0)
                drain(indexer(1))
                for qb in range(NQB):
                    if qb + 1 < NQB:
                        bisect(qb + 1)
                    masktr(qb)
                    interleave(attend(qb), indexer(qb + 2) if qb + 2 < NQB else iter(()))

                n = 0
                for j in range(4):
                    for tt in range(4):
                        cs = slice(tt * 512, (tt + 1) * 512)
                        p = pb[n % 2]
                        o = ob[n % 2]
                        n += 1
                        for par in range(2):
                            S.op('pe', lambda e: e.matmul(p[:], lhsT=wuvp[:, 2 * j + par, :], rhs=ctxn[:, 2 * j + par, cs],
                                                          start=(par == 0), stop=(par == 1)), r=[wuvp, ctxn], w=[p])
                        S.op('act', lambda e: e.copy(out=o[:], in_=p[:]), r=[p], w=[o])
                        S.dma('sp', lambda e: e.dma_start(out=self.mixT[b, 256 + j * 128:256 + (j + 1) * 128, cs],
                                                          in_=o[:]), r=[o], w=[("mixT", b, 2 + j, tt)])
        S.barrier()

    def phase_wout(self, l):
        S, nc = self.S, self.nc
        xsrc = self.x if l == 0 else self.xres
        with ExitStack() as es:
            pb = [self.ps(es, f"wb{i}", [128, 512]) for i in range(8)]
            wout = self.sb(es, "wout", [128, 8, D], BF16)
            bcl = self.sb(es, "bcl2", [128, NSEQ, 3, D])
            rw = self.sb(es, "rw", [128, 8, 36])
            rb = self.sb(es, "rb", [1, 36])
            mx = [self.sb(es, f"mx{i}", [128, 8, 512], BF16) for i in range(2)]
            xin = [self.sb(es, f"wx{i}", [128, D]) for i in range(3)]
            x1 = [self.sb(es, f"wx1{i}", [128, D]) for i in range(2)]
            tmp = self.sb(es, "wtmp", [128, D])
            junk = self.sb(es, "wjunk", [128, D], BF16)
            h2 = self.sb(es, "wh2", [128, D])
            h2b = [self.sb(es, f"wh2b{i}", [128, D], BF16) for i in range(2)]
            h2T = self.sb(es, "wh2T", [128, 8, 128])
            ss = self.sb(es, "wss", [128, 1])
            rstd = self.sb(es, "wrstd", [128, 1])
            RL = self.sb(es, "RL", [128, NT, 36])
            S.dma('pool', lambda e: e.dma_start(out=wout[:], in_=self.w_out[l].rearrange("(k p) n -> p k n", p=128)),
                  w=[wout])
            for b in range(NSEQ):
                S.dma('sp', lambda e: e.dma_start(out=bcl[:, b, :, :], in_=self.bcD[:, b, 2:5, :]), w=[bcl])
            S.dma('sp', lambda e: e.dma_start(out=rw[:], in_=self.router_w[l].rearrange("(k p) n -> p k n", p=128)),
                  w=[rw])
            S.dma('sp', lambda e: e.dma_start(out=rb[:], in_=self.router_b[l:l + 1, :]), w=[rb])
            zt = self.sb(es, "hz", [128, 5, D], BF16)
            S.op('pool', lambda e: e.memset(zt[:], 0.0), w=[zt])
            hsv = self.Hs.rearrange("(n p) d -> p n d", p=128)
            zkeys = []
            for j in range(PSLOT // 128 // 5):
                zkeys.append(("Hs0", j))
                S.dma('act', lambda e: e.dma_start(out=hsv[:, j * 5:(j + 1) * 5, :], in_=zt[:]), r=[zt], w=[zkeys[-1]])

            def load_mx(g):
                b_, t0 = g // 4, (g % 4) * 512
                for k in range(8):
                    S.dma('sp', lambda e: e.dma_start(out=mx[g % 2][:, k, :],
                                                      in_=self.mixT[b_, k * 128:(k + 1) * 128, t0:t0 + 512]),
                          w=[(mx[g % 2].name, k)])

            def load_x(i):
                S.dma('sp', lambda e: e.dma_start(out=xin[i % 3][:], in_=xsrc[i * 128:(i + 1) * 128, :]),
                      w=[xin[i % 3]])
            tmp2 = self.sb(es, "wtmp2", [128, D])
            deferred = []

            def s1(i):
                b = i // (NT // NSEQ)
                sub, g = i % 4, i // 4
                mxg = mx[g % 2]
                xi, x1i = xin[i % 3], x1[i % 2]
                for nh in range(2):
                    hs = slice(nh * 512, (nh + 1) * 512)
                    p = pb[nh]
                    for k in range(8):
                        S.op('pe', lambda e: e.matmul(p[:], lhsT=mxg[:, k, sub * 128:(sub + 1) * 128], rhs=wout[:, k, hs],
                                                      start=(k == 0), stop=(k == 7)), r=[(mxg.name, k), wout], w=[p])
                    S.op('dve', lambda e: e.tensor_tensor(out=tmp[:, hs], in0=p[:], in1=bcl[:, b, 0, hs], op=ALU.mult),
                         r=[p, bcl], w=[(tmp.name, nh)])
                    S.op('dve', lambda e: e.tensor_tensor(out=x1i[:, hs], in0=tmp[:, hs], in1=xi[:, hs], op=ALU.add),
                         r=[(tmp.name, nh), xi], w=[(x1i.name, nh)])
                deferred.append(lambda: S.dma(
                    'sp', lambda e: e.dma_start(out=self.xres[i * 128:(i + 1) * 128, :], in_=x1i[:]),
                    r=[(x1i.name, 0), (x1i.name, 1)], w=[("xres", i)]))

            def s2(i):
                b = i // (NT // NSEQ)
                x1i, h2bi = x1[i % 2], h2b[i % 2]
                S.raw('act', r=[(x1i.name, 0), (x1i.name, 1)])
                self.rms_rstd(x1i, junk, ss, rstd, D)
                S.op('dve', lambda e: e.scalar_tensor_tensor(out=tmp2[:], in0=x1i[:], scalar=rstd[:, 0:1],
                                                             in1=bcl[:, b, 2, :], op0=ALU.mult, op1=ALU.mult),
                     r=[(x1i.name, 0), (x1i.name, 1), rstd, bcl], w=[tmp2])
                S.op('pool', lambda e: e.tensor_tensor(out=h2[:], in0=tmp2[:], in1=bcl[:, b, 1, :], op=ALU.add),
                     r=[tmp2, bcl], w=[h2])
                S.op('act', lambda e: e.copy(out=h2bi[:], in_=h2[:]), r=[h2], w=[h2bi])
                deferred.append(lambda: S.dma(
                    'sp', lambda e: e.dma_start(out=self.H2[i * 128:(i + 1) * 128, :], in_=h2bi[:]), r=[h2bi],
                    w=[("H2", i)]))
                for hh in range(2):
                    pv = pb[2 + hh][:].rearrange("p (k t) -> p k t", k=4)
                    for k in range(4):
                        kk = hh * 4 + k
                        S.op('pe', lambda e: e.transpose(pv[:, k, :], h2[:, kk * 128:(kk + 1) * 128], self.ident_f),
                             r=[h2, self.cf], w=[pb[2 + hh]])
                    S.op('act', lambda e: e.copy(out=h2T[:, hh * 4:(hh + 1) * 4, :], in_=pv), r=[pb[2 + hh]],
                         w=[(h2T.name, hh)])
                for k in range(8):
                    S.op('pe', lambda e: e.matmul(pb[4][:, 0:36], lhsT=h2T[:, k, :], rhs=rw[:, k, :], start=(k == 0),
                                                  stop=False), r=[(h2T.name, k // 4), rw], w=[pb[4]])
                S.op('pe', lambda e: e.matmul(pb[4][:, 0:36], lhsT=self.ones_f[0:1, :], rhs=rb[0:1, :], start=False,
                                              stop=True), r=[self.cf, rb], w=[pb[4]])
                S.op('dve', lambda e: e.tensor_copy(out=RL[:, i, :], in_=pb[4][:, 0:36]), r=[pb[4]], w=[RL])

            load_mx(0)
            load_x(0)
            load_x(1)
            s1(0)
            for i in range(NT):
                if i + 2 < NT:
                    load_x(i + 2)
                if (i + 1) % 4 == 1 and (i + 1) // 4 + 1 < NT // 4:
                    load_mx((i + 1) // 4 + 1)
                for fn in deferred:
                    fn()
                deferred.clear()
                if i + 1 < NT:
                    s1(i + 1)
                s2(i)
            for fn in deferred:
                fn()
            V = 'dve'
            sm = self.sb(es, "rsm", [128, 8, NT])
            g4 = self.sb(es, "rg4", [128, NT, 4])
            ohg = self.sb(es, "ohg", [128, NT, 4])
            t48 = self.sb(es, "rt48", [128, NT, 4, 8])
            esel = self.sb(es, "esel", [128, NT, 8])
            esel2 = self.sb(es, "esel2", [128, NT, 8])
            oh = self.sb(es, "oh12", [128, 2, NT, 8])
            Eb = self.sb(es, "Eb", [128, NT * 64], BF16)
            pfx = self.sb(es, "rpfx", [128, NT, 64])
            tot = self.sb(es, "rtot", [128, NT, 64])
            ts_ = [self.sb(es, f"rts{i}", [128, NT, 32]) for i in range(2)]
            tsum = self.sb(es, "rtsum", [128, NT, 32])
            sc_ = [self.sb(es, f"scn{i}", [128, 32]) for i in range(2)]
            pci = self.sb(es, "pci", [128, 32], I32)
            pstart = self.sb(es, "pstart", [128, 32])
            bacc = self.sb(es, "bacc", [128, NBLK])
            boob = self.sb(es, "boob", [128, NBLK])
            dtmp = self.sb(es, "dtmp", [128, NT, 2, 32])
            dst = self.sb(es, "dstf", [128, NT, 2])
            lg = RL[:, :, 0:4]
            le = RL[:, :, 4:36].rearrange("p t (g e) -> p t g e", g=4)

            def bc3(ap, n):
                return ap.unsqueeze(2).to_broadcast([128, NT, n])
            S.op(V, lambda e: e.tensor_reduce(out=sm[:, 0, :], in_=lg, axis=AX.X, op=ALU.max), r=[RL], w=[sm])
            S.op(V, lambda e: e.tensor_tensor(out=ohg[:], in0=lg, in1=bc3(sm[:, 0, :], 4), op=ALU.is_equal),
                 r=[RL, sm], w=[ohg])
            S.op(V, lambda e: e.tensor_tensor(out=g4[:], in0=lg, in1=bc3(sm[:, 0, :], 4), op=ALU.subtract),
                 r=[RL, sm], w=[g4])
            S.op('act', lambda e: e.activation(out=g4[:], in_=g4[:], func=AF.Exp), r=[g4], w=[g4])
            S.op(V, lambda e: e.tensor_reduce(out=sm[:, 1, :], in_=g4[:], axis=AX.X, op=ALU.add), r=[g4], w=[sm])
            S.op(V, lambda e: e.reciprocal(out=sm[:, 2, :], in_=sm[:, 1, :]), r=[sm], w=[sm])
            S.op(V, lambda e: e.tensor_tensor(out=t48[:], in0=le, in1=ohg[:].unsqueeze(3).to_broadcast([128, NT, 4, 8]),
                                              op=ALU.mult), r=[RL, ohg], w=[t48])
            S.op(V, lambda e: e.tensor_reduce(out=esel[:], in_=t48[:].rearrange("p t g e -> p t e g"), axis=AX.X,
                                              op=ALU.add), r=[t48], w=[esel])
            S.op(V, lambda e: e.tensor_reduce(out=sm[:, 3, :], in_=esel[:], axis=AX.X, op=ALU.max), r=[esel], w=[sm])
            S.op(V, lambda e: e.tensor_tensor(out=oh[:, 0, :, :], in0=esel[:], in1=bc3(sm[:, 3, :], 8), op=ALU.is_equal),
                 r=[esel, sm], w=[oh])
            S.op(V, lambda e: e.scalar_tensor_tensor(out=esel2[:], in0=oh[:, 0, :, :], scalar=-1e30, in1=esel[:],
                                                     op0=ALU.mult, op1=ALU.add), r=[oh, esel], w=[esel2])
            S.op(V, lambda e: e.tensor_reduce(out=sm[:, 4, :], in_=esel2[:], axis=AX.X, op=ALU.max), r=[esel2], w=[sm])
            S.op(V, lambda e: e.tensor_tensor(out=oh[:, 1, :, :], in0=esel2[:], in1=bc3(sm[:, 4, :], 8),
                                              op=ALU.is_equal), r=[esel2, sm], w=[oh])
            S.op(V, lambda e: e.tensor_tensor(out=sm[:, 5, :], in0=sm[:, 3, :], in1=sm[:, 4, :], op=ALU.subtract),
                 r=[sm], w=[sm])
            S.op('act', lambda e: e.activation(out=sm[:, 6, :], in_=sm[:, 5, :], func=AF.Sigmoid), r=[sm], w=[sm])
            S.op(V, lambda e: e.tensor_tensor(out=self.GT[:, :, 0], in0=sm[:, 6, :], in1=sm[:, 2, :], op=ALU.mult),
                 r=[sm], w=["GT"])
            S.op(V, lambda e: e.tensor_tensor(out=self.GT[:, :, 1], in0=sm[:, 2, :], in1=self.GT[:, :, 0],
                                              op=ALU.subtract), r=[sm, "GT"], w=["GT"])
            Ev = self.EALL[:].rearrange("p t (k g e) -> p t k g e", k=2, g=4)
            for k in range(2):
                S.op(V, lambda e: e.tensor_tensor(out=Ev[:, :, k, :, :],
                                                  in0=ohg[:].unsqueeze(3).to_broadcast([128, NT, 4, 8]),
                                                  in1=oh[:, k, :, :].unsqueeze(2).to_broadcast([128, NT, 4, 8]),
                                                  op=ALU.mult), r=[ohg, oh], w=["EALL"])
            S.op('act', lambda e: e.copy(out=Eb[:], in_=self.EALL[:].rearrange("p t c -> p (t c)")), r=["EALL"], w=[Eb])
            for j in range(4):
                S.op('pe', lambda e: e.matmul(pb[j][:], lhsT=self.triu_b, rhs=Eb[:, j * 512:(j + 1) * 512], start=True,
                                              stop=True), r=[self.cb, Eb], w=[pb[j]])
                S.op('pe', lambda e: e.matmul(pb[4 + j][:], lhsT=self.ones_b, rhs=Eb[:, j * 512:(j + 1) * 512],
                                              start=True, stop=True), r=[self.cb, Eb], w=[pb[4 + j]])
                S.op('act', lambda e: e.copy(out=pfx[:, j * 8:(j + 1) * 8, :].rearrange("p t c -> p (t c)"),
                                             in_=pb[j][:]), r=[pb[j]], w=[pfx])
                S.op(V, lambda e: e.tensor_copy(out=tot[:, j * 8:(j + 1) * 8, :].rearrange("p t c -> p (t c)"),
                                                in_=pb[4 + j][:]), r=[pb[4 + j]], w=[tot])
            S.op(V, lambda e: e.tensor_tensor(out=tsum[:], in0=tot[:, :, 0:32], in1=tot[:, :, 32:64], op=ALU.add),
                 r=[tot], w=[tsum])
            S.op(V, lambda e: e.tensor_copy(out=ts_[0][:], in_=tsum[:]), r=[tsum], w=[ts_[0]])
            cur = 0
            shf = 1
            while shf < NT:
                a, bb = ts_[cur], ts_[1 - cur]
                S.op(V, lambda e: e.tensor_copy(out=bb[:, 0:shf, :], in_=a[:, 0:shf, :]), r=[a], w=[bb])
                S.op(V, lambda e: e.tensor_tensor(out=bb[:, shf:NT, :], in0=a[:, shf:NT, :], in1=a[:, 0:NT - shf, :],
                                                  op=ALU.add), r=[a, bb], w=[bb])
                cur = 1 - cur
                shf *= 2
            incl = ts_[cur]
            base = ts_[1 - cur]
            S.op(V, lambda e: e.tensor_tensor(out=base[:], in0=incl[:], in1=tsum[:], op=ALU.subtract),
                 r=[incl, tsum], w=[base])
            for k in range(2):
                S.op(V, lambda e: e.tensor_tensor(out=pfx[:, :, k * 32:(k + 1) * 32], in0=pfx[:, :, k * 32:(k + 1) * 32],
                                                  in1=base[:], op=ALU.add), r=[pfx, base], w=[pfx])
            S.op(V, lambda e: e.tensor_tensor(out=pfx[:, :, 32:64], in0=pfx[:, :, 32:64], in1=tot[:, :, 0:32],
                                              op=ALU.add), r=[pfx, tot], w=[pfx])
            S.op(V, lambda e: e.tensor_tensor(out=pfx[:], in0=pfx[:], in1=self.EALL[:], op=ALU.mult),
                 r=[pfx, "EALL"], w=[pfx])
            S.op(V, lambda e: e.tensor_reduce(out=self.RK[:], in_=pfx[:].rearrange("p t (k e) -> p t k e", k=2),
                                              axis=AX.X, op=ALU.add), r=[pfx], w=["RK"])
            sh = MB.bit_length() - 1
            S.op(V, lambda e: e.tensor_scalar(out=sc_[0][:], in0=incl[:, NT - 1, :], scalar1=float(MB - 1),
                                              scalar2=None, op0=ALU.add), r=[incl], w=[sc_[0]])
            S.op(V, lambda e: e.tensor_copy(out=pci[:], in_=sc_[0][:]), r=[sc_[0]], w=[pci])
            S.op(V, lambda e: e.tensor_scalar(out=pci[:], in0=pci[:], scalar1=sh, scalar2=sh,
                                              op0=ALU.arith_shift_right, op1=ALU.logical_shift_left), r=[pci], w=[pci])
            S.op(V, lambda e: e.tensor_copy(out=sc_[0][:], in_=pci[:]), r=[pci], w=[sc_[0]])
            S.op(V, lambda e: e.tensor_copy(out=pstart[:], in_=sc_[0][:]), r=[sc_[0]], w=[pstart])
            cur = 0
            for shf in (1, 2, 4, 8, 16):
                a, bb = sc_[cur], sc_[1 - cur]
                S.op(V, lambda e: e.tensor_copy(out=bb[:, 0:shf], in_=a[:, 0:shf]), r=[a], w=[bb])
                S.op(V, lambda e: e.tensor_tensor(out=bb[:, shf:32], in0=a[:, shf:32], in1=a[:, 0:32 - shf], op=ALU.add),
                     r=[a, bb], w=[bb])
                cur = 1 - cur
            pend = sc_[cur]
            S.op(V, lambda e: e.tensor_tensor(out=pstart[:], in0=pend[:], in1=pstart[:], op=ALU.subtract),
                 r=[pend, pstart], w=[pstart])
            S.op('pool', lambda e: e.memset(bacc[:], 0.0), w=[bacc])
            for ex in range(NEXP):
                S.op(V, lambda e: e.scalar_tensor_tensor(out=bacc[:], in0=self.cf[:, 512:512 + NBLK],
                                                         scalar=pend[:, ex:ex + 1], in1=bacc[:], op0=ALU.is_ge,
                                                         op1=ALU.add), r=[self.cf, pend, bacc], w=[bacc])
            S.op(V, lambda e: e.tensor_scalar(out=boob[:], in0=bacc[:], scalar1=float(NEXP), scalar2=1.0e6,
                                              op0=ALU.is_ge, op1=ALU.mult), r=[bacc], w=[boob])
            S.op(V, lambda e: e.tensor_scalar(out=bacc[:], in0=bacc[:], scalar1=float(NEXP - 1), scalar2=None,
                                              op0=ALU.min), r=[bacc], w=[bacc])
            S.op(V, lambda e: e.tensor_scalar(out=bacc[:], in0=bacc[:], scalar1=128.0, scalar2=self.cf[:, 256:257],
                                              op0=ALU.mult, op1=ALU.add), r=[bacc, self.cf], w=[bacc])
            S.op(V, lambda e: e.tensor_tensor(out=bacc[:], in0=bacc[:], in1=boob[:], op=ALU.add), r=[bacc, boob],
                 w=[bacc])
            S.op(V, lambda e: e.tensor_copy(out=self.BEXP[:], in_=bacc[:]), r=[bacc], w=[self.BEXP])
            S.op(V, lambda e: e.tensor_tensor(out=dtmp[:], in0=self.EALL[:].rearrange("p t (k e) -> p t k e", k=2),
                                              in1=pstart[:].unsqueeze(1).unsqueeze(1).to_broadcast([128, NT, 2, 32]),
                                              op=ALU.mult), r=["EALL", pstart], w=[dtmp])
            S.op(V, lambda e: e.tensor_reduce(out=dst[:], in_=dtmp[:], axis=AX.X, op=ALU.add), r=[dtmp], w=[dst])
            S.op(V, lambda e: e.tensor_tensor(out=dst[:], in0=dst[:], in1=self.RK[:], op=ALU.add), r=[dst, "RK"],
                 w=[dst])
            S.op(V, lambda e: e.tensor_copy(out=self.DSTI[:], in_=dst[:]), r=[dst], w=["DSTI"])
            S.raw('pool', r=zkeys)
            h2s = [self.sb(es, f"wh2s{i}", [128, D], BF16) for i in range(6)]
            for i in range(NT):
                h2bi = h2s[i % 6]
                S.dma('sp', lambda e: e.dma_start(out=h2bi[:], in_=self.H2[i * 128:(i + 1) * 128, :]), r=[("H2", i)],
                      w=[h2bi])
                for k in range(2):
                    S.dma('pool', lambda e: e.indirect_dma_start(
                        out=self.Hs[:, :], out_offset=bass.IndirectOffsetOnAxis(ap=self.DSTI[:, i, k:k + 1], axis=0),
                        in_=h2bi[:], in_offset=None), r=[h2bi, "DSTI"], w=["Hs"])
        S.barrier()

    def phase_experts(self, l):
        S, nc = self.S, self.nc
        NSUB = MB // 128
        with ExitStack() as es:
            pb = [self.ps(es, f"eb{i}", [128, 512]) for i in range(8)]
            w1b = [self.sb(es, f"w1b{i}", [128, 8, DEXP], BF16) for i in range(3)]
            w3b = [self.sb(es, f"w3b{i}", [128, 8, DEXP], BF16) for i in range(3)]
            w2b = [self.sb(es, f"w2b{i}", [128, 4, D], BF16) for i in range(3)]
            hb = [self.sb(es, f"ehb{i}", [128, NSUB, D], BF16) for i in range(2)]
            hbT = [self.sb(es, f"ehbT{i}", [128, 8, MB], BF16) for i in range(2)]
            sa = [self.sb(es, f"esa{i}", [128, MB]) for i in range(2)]
            abT = [self.sb(es, f"eabT{i}", [128, 4, MB], BF16) for i in range(2)]
            ysb = [self.sb(es, f"eys{i}", [128, D]) for i in range(4)]
            ny = 0
            npp = 0
            deferred = []
            if not hasattr(self, "oob_reg"):
                self.oob_reg = nc.gpsimd.to_reg(NEXP * 128 - 1)
            for bk in range(NBLK):
                pi = bk % 2
                wi3 = bk % 3
                for wsrc, wdst, kh in ((self.exp_w1, w1b[wi3], 4), (self.exp_w3, w3b[wi3], 4), (self.exp_w2, w2b[wi3], 2)):
                    for h in range(2):
                        S.dma('pool', lambda e: e.indirect_dma_start(
                            out=wdst[:, h * kh:(h + 1) * kh, :].rearrange("p k f -> p (k f)"), out_offset=None,
                            in_=wsrc[l][h][:, :],
                            in_offset=bass.IndirectOffsetOnAxis(ap=self.BEXP[:, bk:bk + 1], axis=0),
                            bounds_check=self.oob_reg, oob_is_err=False),
                            r=[self.BEXP], w=[(wdst.name, h)])
                if bk == 0:
                    S.dma('sp', lambda e: e.dma_start(
                        out=hb[0][:], in_=self.Hs[0:MB, :].rearrange("(s p) d -> p s d", p=128)), r=["Hs"], w=[hb[0]])
                if bk + 1 < NBLK:
                    S.dma('sp', lambda e: e.dma_start(
                        out=hb[1 - pi][:],
                        in_=self.Hs[(bk + 1) * MB:(bk + 2) * MB, :].rearrange("(s p) d -> p s d", p=128)),
                        r=["Hs"], w=[hb[1 - pi]])
                for fn in deferred:
                    fn()
                deferred = []
                def tr(bb):
                    pj = bb % 2
                    for sub in range(NSUB):
                        tpv = pb[sub][:].bitcast(BF16).rearrange("p (k t) -> p k t", k=8)
                        for k in range(8):
                            S.op('pe', lambda e: e.transpose(tpv[:, k, :], hb[pj][:, sub, k * 128:(k + 1) * 128],
                                                             self.ident_b), r=[hb[pj], self.cb], w=[pb[sub]])
                        S.op('act', lambda e: e.copy(out=hbT[pj][:, :, sub * 128:(sub + 1) * 128], in_=tpv),
                             r=[pb[sub]], w=[(hbT[pj].name, sub)])
                if bk == 0:
                    tr(0)
                for fc in range(4):
                    pa, pg = pb[2 + (npp % 2) * 2], pb[3 + (npp % 2) * 2]
                    sai = sa[npp % 2]
                    npp += 1
                    for k in range(8):
                        S.op('pe', lambda e: e.matmul(pa[:, 0:MB], lhsT=w1b[wi3][:, k, fc * 128:(fc + 1) * 128],
                                                      rhs=hbT[pi][:, k, :], start=(k == 0), stop=(k == 7)),
                             r=[(w1b[wi3].name, 0), (w1b[wi3].name, 1), (hbT[pi].name, 0), (hbT[pi].name, 1)], w=[pa])
                    for k in range(8):
                        S.op('pe', lambda e: e.matmul(pg[:, 0:MB], lhsT=w3b[wi3][:, k, fc * 128:(fc + 1) * 128],
                                                      rhs=hbT[pi][:, k, :], start=(k == 0), stop=(k == 7)),
                             r=[(w3b[wi3].name, 0), (w3b[wi3].name, 1), (hbT[pi].name, 0), (hbT[pi].name, 1)], w=[pg])
                    S.op('act', lambda e: e.activation(out=sai[:], in_=pa[:, 0:MB], func=AF.Silu), r=[pa], w=[sai])
                    S.op('dve', lambda e: e.tensor_tensor(out=abT[pi][:, fc, :], in0=pg[:, 0:MB], in1=sai[:], op=ALU.mult),
                         r=[pg, sai], w=[(abT[pi].name, fc)])
                if bk + 1 < NBLK:
                    tr(bk + 1)
                for sub in range(NSUB):
                    y = ysb[ny % 4]
                    ny += 1
                    for nh in range(2):
                        p = pb[6 + nh]
                        for kf in range(4):
                            S.op('pe', lambda e: e.matmul(p[:], lhsT=abT[pi][:, kf, sub * 128:(sub + 1) * 128],
                                                          rhs=w2b[wi3][:, kf, nh * 512:(nh + 1) * 512], start=(kf == 0),
                                                          stop=(kf == 3)), r=[(abT[pi].name, kf), (w2b[wi3].name, 0), (w2b[wi3].name, 1)], w=[p])
                        if nh == 0:
                            S.op('act', lambda e: e.copy(out=y[:, 0:512], in_=p[:]), r=[p], w=[(y.name, 0)])
                        else:
                            S.op('dve', lambda e: e.tensor_copy(out=y[:, 512:1024], in_=p[:]), r=[p], w=[(y.name, 1)])
                    r0 = bk * MB + sub * 128
                    deferred.append(lambda y=y, r0=r0, bk=bk, sub=sub: S.dma(
                        'sp', lambda e: e.dma_start(out=self.Ys[r0:r0 + 128, :], in_=y[:]),
                        r=[(y.name, 0), (y.name, 1)], w=[("Ys", bk, sub)]))
            for fn in deferred:
                fn()
        S.barrier()

    def phase_combine(self, l, last):
        S, nc = self.S, self.nc
        NB = 4
        with ExitStack() as es:
            g2 = self.sb(es, "g2bc", [128, NSEQ, D])
            fg = self.sb(es, "fgbc", [128, D])
            xin = [self.sb(es, f"cx{i}", [128, D]) for i in range(NB)]
            y0 = [self.sb(es, f"cy0{i}", [128, D]) for i in range(NB)]
            y1 = [self.sb(es, f"cy1{i}", [128, D]) for i in range(NB)]
            mo = [self.sb(es, f"cmo{i}", [128, D]) for i in range(2)]
            x2 = [self.sb(es, f"cx2{i}", [128, D]) for i in range(NB)]
            junk = self.sb(es, "cjunk", [128, D], BF16)
            ss = [self.sb(es, f"css{i}", [128, 1]) for i in range(2)]
            rstd = [self.sb(es, f"crstd{i}", [128, 1]) for i in range(2)]
            for b in range(NSEQ):
                S.dma('sp', lambda e: e.dma_start(out=g2[:, b, :], in_=self.bcD[:, b, 5, :]), w=[g2])
            if last:
                S.dma('sp', lambda e: e.dma_start(out=fg[:], in_=self.final_g[0:1, :].partition_broadcast(128)), w=[fg])

            def fetch(i):
                S.dma('sp', lambda e: e.dma_start(out=xin[i % NB][:], in_=self.xres[i * 128:(i + 1) * 128, :]),
                      r=[("xres", i)], w=[xin[i % NB]])
                for k, yy in ((0, y0[i % NB]), (1, y1[i % NB])):
                    S.dma('pool', lambda e: e.indirect_dma_start(
                        out=yy[:], out_offset=None, in_=self.Ys[:, :],
                        in_offset=bass.IndirectOffsetOnAxis(ap=self.DSTI[:, i, k:k + 1], axis=0)), r=["DSTI"], w=[yy])
            fetch(0)
            fetch(1)
            deferred = []
            for i in range(NT):
                b = i // (NT // NSEQ)
                if i + 2 < NT:
                    fetch(i + 2)
                for fn in deferred:
                    fn()
                deferred = []
                xi, y0i, y1i, x2i, moi = xin[i % NB], y0[i % NB], y1[i % NB], x2[i % NB], mo[i % 2]
                S.op('act', lambda e: e.mul(out=moi[:], in_=y0i[:], mul=self.GT[:, i, 0:1]), r=[y0i, "GT"], w=[moi])
                S.op('dve', lambda e: e.scalar_tensor_tensor(out=moi[:], in0=y1i[:], scalar=self.GT[:, i, 1:2], in1=moi[:],
                                                             op0=ALU.mult, op1=ALU.add), r=[y1i, moi, "GT"], w=[moi])
                S.op('dve', lambda e: e.tensor_tensor(out=moi[:], in0=moi[:], in1=g2[:, b, :], op=ALU.mult),
                     r=[moi, g2], w=[moi])
                S.op('dve', lambda e: e.tensor_tensor(out=x2i[:], in0=moi[:], in1=xi[:], op=ALU.add), r=[moi, xi],
                     w=[x2i])
                if not last:
                    deferred.append(lambda i=i, x2i=x2i: S.dma(
                        'sp', lambda e: e.dma_start(out=self.xres[i * 128:(i + 1) * 128, :], in_=x2i[:]), r=[x2i],
                        w=[("xres", i)]))
                else:
                    self.rms_rstd(x2i, junk, ss[i % 2], rstd[i % 2], D)
                    rs = rstd[i % 2]
                    S.op('dve', lambda e: e.scalar_tensor_tensor(out=x2i[:], in0=x2i[:], scalar=rs[:, 0:1], in1=fg[:],
                                                                 op0=ALU.mult, op1=ALU.mult), r=[x2i, rs, fg],
                         w=[x2i])
                    deferred.append(lambda i=i, x2i=x2i: S.dma(
                        'sp', lambda e: e.dma_start(out=self.out[i * 128:(i + 1) * 128, :], in_=x2i[:]), r=[x2i],
                        w=[("out", i)]))
            for fn in deferred:
                fn()
        S.barrier()

    def build_all(self):
        self.declare()
        self.setup()
        for l in range(self.nlayers):
            self.phase_mod(l)
            self.phase_front(l)
            self.phase_conv(l)
            self.phase_pool(l)
            self.phase_attn(l)
            self.phase_wout(l)
            self.phase_experts(l)
            self.phase_combine(l, last=(l == self.nlayers - 1))
        self.S.barrier()
        self.es.close()
        return self.nc


def _wlay(w, kh):
    w = np.asarray(w, dtype=np.float32)
    L, E, R, Fd = w.shape
    w = w.reshape(L, E, 2, kh, 128, Fd).transpose(0, 2, 1, 4, 3, 5)
    return np.ascontiguousarray(w).reshape(L, 2, E * 128, kh * Fd)


def host_inputs(inputs):
    f = lambda a: np.ascontiguousarray(np.asarray(a, dtype=np.float32))
    cf, cb, pinv = make_consts()
    L = DEPTH
    shared = {
        "mod_w": f(inputs["mod_w"]), "mod_b": f(inputs["mod_b"]), "norm1_g": f(inputs["norm1_g"]),
        "w_in": f(inputs["w_in"]), "conv_k": f(inputs["conv_k"]), "conv_b": f(inputs["conv_b"]),
        "conv_ln_g": f(inputs["conv_ln_g"]), "conv_ln_b": f(inputs["conv_ln_b"]),
        "q_norm_g": f(inputs["q_norm_g"]), "kv_norm_g": f(inputs["kv_norm_g"]),
        "w_uq": f(inputs["w_uq"]).reshape(L, 256, 512), "w_uk": f(inputs["w_uk"]).reshape(L, 128, 512),
        "w_uv": f(inputs["w_uv"]).reshape(L, 128, 512), "pool_w": f(inputs["pool_w"]),
        "pool_scale": f(inputs["pool_scale"]), "w_out": f(inputs["w_out"]), "norm2_g": f(inputs["norm2_g"]),
        "router_w": np.ascontiguousarray(np.concatenate(
            [f(inputs["router_g_w"]), f(inputs["router_e_w"]).reshape(L, D, 32)], axis=2)),
        "router_b": np.ascontiguousarray(np.concatenate(
            [f(inputs["router_g_b"]), f(inputs["router_e_b"]).reshape(L, 32)], axis=1)),
        "final_g": f(inputs["final_g"]).reshape(1, D), "cf": cf, "cb": cb, "pinv": pinv,
    }
    for nm, kh in (("exp_w1", 4), ("exp_w3", 4), ("exp_w2", 2)):
        wl = _wlay(inputs[nm], kh)
        for a in range(L):
            for h in range(2):
                shared[f"{nm}_{a}_{h}"] = np.ascontiguousarray(wl[a, h])
    x = f(inputs["x"])
    c = f(inputs["c"])
    maps = []
    for i in range(NCORES):
        m = dict(shared)
        m["x"] = np.ascontiguousarray(x[i * NSEQ:(i + 1) * NSEQ].reshape(T, D))
        m["c"] = np.ascontiguousarray(c[i * NSEQ:(i + 1) * NSEQ])
        maps.append(m)
    return maps


_CACHE = {}


def kernel(**inputs):
    maps = host_inputs(inputs)
    if "nc" not in _CACHE:
        _CACHE["nc"] = Prog().build_all()
    res = run_bass_kernel_spmd(_CACHE["nc"], maps, core_ids=list(range(NCORES)))
    outs = [np.asarray(r["out"], dtype=np.float32).reshape(NSEQ, SEQ, D) for r in res.results]
    return np.concatenate(outs, axis=0)
```

```python
import numpy as np
import ml_dtypes
from contextlib import ExitStack
import concourse.bass as bass
import concourse.mybir as mybir
from concourse.bass_utils import run_bass_kernel_spmd

F32 = mybir.dt.float32
BF16 = mybir.dt.bfloat16
I32 = mybir.dt.int32
U32 = mybir.dt.uint32
AF = mybir.ActivationFunctionType
ALU = mybir.AluOpType
AX = mybir.AxisListType

NCORES = 8
D = 1024
SEQ = 2048
NSEQ = 2
T = NSEQ * SEQ
NT = T // 128
DEPTH = 2
N_IN = 1736
EPS = 1e-6
NEXP = 32
DEXP = 512
TOPK = 256
MB = 256
NBLK = (2 * T + NEXP * (MB - 1)) // MB + 1
PSLOT = NBLK * MB


class Sched:
    def __init__(self, nc, es, ndma=8, same_engine_sync=True):
        self.nc = nc
        self.E = {'pe': nc.tensor, 'act': nc.scalar, 'dve': nc.vector, 'pool': nc.gpsimd, 'sp': nc.sync}
        self.same = same_engine_sync
        self.csem = {}
        self.ccnt = {}
        for e in ['pe', 'act', 'dve', 'pool']:
            self.csem[e] = es.enter_context(nc.semaphore(f"c_{e}"))
            self.ccnt[e] = 0
        self.dsem = {}
        self.dtot = {}
        self.dnext = {}
        for q in ['sp', 'pool', 'act']:
            self.dsem[q] = [es.enter_context(nc.semaphore(f"d_{q}_{i}")) for i in range(ndma)]
            self.dtot[q] = [0] * ndma
            self.dnext[q] = 0
        self.waited = {e: {} for e in self.E}
        self.res = {}
        self.nwait = 0
        self.nins = 0

    @staticmethod
    def _key(x):
        return x if isinstance(x, (str, tuple)) else x.name

    def _wait(self, eng, tok):
        sem, sname, val, src = tok
        if src == eng:
            if eng == 'pe' or not self.same:
                return
        if self.waited[eng].get(sname, 0) >= val:
            return
        self.E[eng].wait_ge(sem, val)
        self.waited[eng][sname] = val
        self.nwait += 1

    def _deps(self, eng, r, w):
        for k in r:
            st = self.res.get(self._key(k))
            if st and st[0] is not None:
                self._wait(eng, st[0])
        for k in w:
            st = self.res.get(self._key(k))
            if st:
                if st[0] is not None:
                    self._wait(eng, st[0])
                for t in st[1].values():
                    self._wait(eng, t)

    def _commit(self, tok, r, w):
        for k in w:
            self.res[self._key(k)] = [tok, {}]
        for k in r:
            st = self.res.setdefault(self._key(k), [None, {}])
            st[1][tok[1]] = tok

    def op(self, eng, fn, r=(), w=()):
        self._deps(eng, r, w)
        ins = fn(self.E[eng])
        self.ccnt[eng] += 1
        ins.then_inc(self.csem[eng], 1)
        tok = (self.csem[eng], 'c_' + eng, self.ccnt[eng], eng)
        self._commit(tok, r, w)
        self.nins += 1
        return tok

    def dma(self, q, fn, r=(), w=()):
        self._deps(q, r, w)
        i = self.dnext[q]
        self.dnext[q] = (i + 1) % len(self.dsem[q])
        sem = self.dsem[q][i]
        sname = f"d_{q}_{i}"
        if self.dtot[q][i] > 0:
            self._wait(q, (sem, sname, self.dtot[q][i], 'dma'))
        ins = fn(self.E[q])
        self.dtot[q][i] += 16
        ins.then_inc(sem, 16)
        tok = (sem, sname, self.dtot[q][i], 'dma')
        self._commit(tok, r, w)
        self.nins += 1
        return tok

    def raw(self, eng, r=(), w=()):
        self._deps(eng, r, w)

    def barrier(self):
        toks = []
        for e, s in self.csem.items():
            if self.ccnt[e]:
                toks.append((s, 'c_' + e, self.ccnt[e], 'x'))
        for q in self.dsem:
            for i, s in enumerate(self.dsem[q]):
                if self.dtot[q][i]:
                    toks.append((s, f"d_{q}_{i}", self.dtot[q][i], 'dma'))
        for e in self.E:
            for t in toks:
                self._wait(e, t)
        self.res = {}


def _bf16(a):
    return np.asarray(a, dtype=np.float32).astype(ml_dtypes.bfloat16)


def make_consts():
    cf = np.zeros((128, 1024), np.float32)
    cf[:, 0:128] = np.eye(128)
    cf[:, 128:256] = 1.0
    cf[:, 256] = np.arange(128)
    cf[0, 640:768] = 1.0
    cf[1, 768:896] = 1.0
    cf[:, 896:896 + 32] = (2.0 ** -(np.arange(32) + 1.0))[None, :]
    cf[:, 384:384 + 32] = np.arange(32)[None, :]
    cf[:, 512:512 + NBLK] = (np.arange(NBLK) * MB)[None, :]
    cb = np.zeros((128, 512), np.float32)
    cb[:, 0:128] = np.eye(128)
    cb[:, 128:256] = 1.0
    cb[:, 256:384] = np.triu(np.ones((128, 128)), 1)
    inv = np.zeros((128, 2, SEQ), np.float32)
    n = np.arange(1, SEQ + 1, dtype=np.float32)
    for g, wdw in enumerate((2, 4, 8, 16)):
        ch, half = divmod(g, 2)
        inv[half * 64:(half + 1) * 64, ch, :] = (1.0 / np.minimum(n, wdw))[None, :]
    return cf, _bf16(cb), inv


class Prog:
    def __init__(self, nlayers=DEPTH, phases=None, debug=False, same_engine_sync=True):
        self.nlayers = nlayers
        self.debug = debug
        self.phases = phases
        self.nc = nc = bass.Bass("TRN2", target_bir_lowering=False)
        self.es = ExitStack()
        self.S = Sched(nc, self.es, same_engine_sync=same_engine_sync)
        self.inp = {}
        self.outs = []

    def din(self, name, shape, dt=F32):
        t = self.nc.dram_tensor(name, list(shape), dt, kind="ExternalInput").ap()
        self.inp[name] = t
        return t

    def dscr(self, name, shape, dt=F32, out=False):
        kind = "ExternalOutput" if (out or self.debug) else "Internal"
        t = self.nc.dram_tensor(name, list(shape), dt, kind=kind).ap()
        if kind == "ExternalOutput":
            self.outs.append(name)
        return t

    def sb(self, es, name, shape, dt=F32):
        self.uid = getattr(self, "uid", 0) + 1
        return es.enter_context(self.nc.sbuf_tensor(f"{name}_u{self.uid}", list(shape), dt))

    def ps(self, es, name, shape, dt=F32):
        self.uid = getattr(self, "uid", 0) + 1
        return es.enter_context(self.nc.psum_tensor(f"{name}_u{self.uid}", list(shape), dt))

    def declare(self):
        L = DEPTH
        d = self.din
        self.x = d("x", [T, D])
        self.c = d("c", [NSEQ, D])
        self.mod_w = d("mod_w", [L, D, 6 * D])
        self.mod_b = d("mod_b", [L, 6 * D])
        self.norm1_g = d("norm1_g", [L, D])
        self.w_in = d("w_in", [L, D, N_IN])
        self.conv_k = d("conv_k", [L, 31, 256])
        self.conv_b = d("conv_b", [L, 256])
        self.conv_ln_g = d("conv_ln_g", [L, 256])
        self.conv_ln_b = d("conv_ln_b", [L, 256])
        self.q_norm_g = d("q_norm_g", [L, 256])
        self.kv_norm_g = d("kv_norm_g", [L, 128])
        self.w_uq = d("w_uq", [L, 256, 512])
        self.w_uk = d("w_uk", [L, 128, 512])
        self.w_uv = d("w_uv", [L, 128, 512])
        self.pool_w = d("pool_w", [L, 4, 64, 64])
        self.pool_scale = d("pool_scale", [L, 256])
        self.w_out = d("w_out", [L, D, D])
        self.norm2_g = d("norm2_g", [L, D])
        self.router_w = d("router_w", [L, D, 36])
        self.router_b = d("router_b", [L, 36])
        self.exp_w1 = [[d(f"exp_w1_{a}_{h}", [NEXP * 128, 2048]) for h in range(2)] for a in range(L)]
        self.exp_w3 = [[d(f"exp_w3_{a}_{h}", [NEXP * 128, 2048]) for h in range(2)] for a in range(L)]
        self.exp_w2 = [[d(f"exp_w2_{a}_{h}", [NEXP * 128, 2048]) for h in range(2)] for a in range(L)]
        self.final_g = d("final_g", [1, D])
        self.cf_d = d("cf", [128, 1024])
        self.cb_d = d("cb", [128, 512], BF16)
        self.pinv_d = d("pinv", [128, 2, SEQ])
        self.out = self.nc.dram_tensor("out", [T, D], F32, kind="ExternalOutput").ap()
        s = self.dscr
        self.xres = s("xres", [T, D])
        self.zA = s("zA", [NSEQ, 512, SEQ])
        self.zCQ = s("zCQ", [NSEQ, 256, SEQ])
        self.zCKV = s("zCKV", [NSEQ, 128, SEQ])
        self.zQI = s("zQI", [NSEQ, 512, SEQ], BF16)
        self.zKI = s("zKI", [NSEQ, 64, SEQ], BF16)
        self.zWI = s("zWI", [NSEQ, SEQ, 8])
        self.zUP = s("zUP", [NSEQ, 256, SEQ])
        self.mixT = s("mixT", [NSEQ, D, SEQ], BF16)
        self.bcD = s("bcD", [128, NSEQ, 6, D])
        self.H2 = s("H2", [T, D], BF16)
        self.Hs = s("Hs", [PSLOT, D], BF16)
        self.Ys = s("Ys", [PSLOT, D])

    def setup(self):
        S, es = self.S, self.es
        self.cf = self.sb(es, "cf_sb", [128, 1024])
        self.cb = self.sb(es, "cb_sb", [128, 512], BF16)
        self.condT = self.sb(es, "condT", [128, 8, NSEQ])
        self.EALL = self.sb(es, "EALL", [128, NT, 64])
        self.RK = self.sb(es, "RK", [128, NT, 2])
        self.GT = self.sb(es, "GT", [128, NT, 2])
        self.DSTI = self.sb(es, "DSTI", [128, NT, 2], I32)
        self.BEXP = self.sb(es, "BEXP", [128, NBLK], I32)
        S.dma('sp', lambda e: e.dma_start(out=self.cf[:], in_=self.cf_d[:, :]), w=[self.cf])
        S.dma('sp', lambda e: e.dma_start(out=self.cb[:], in_=self.cb_d[:, :]), w=[self.cb])
        with self.nc.allow_non_contiguous_dma(reason="tiny transposed load of c"):
            for b in range(NSEQ):
                S.dma('sp', lambda e: e.dma_start(
                    out=self.condT[:, :, b:b + 1],
                    in_=self.c[b:b + 1, :].rearrange("b (k p) -> p k b", p=128)), w=[self.condT])
        S.op('act', lambda e: e.activation(out=self.condT[:], in_=self.condT[:], func=AF.Silu),
             r=[self.condT], w=[self.condT])
        self.ident_f = self.cf[:, 0:128]
        self.ones_f = self.cf[:, 128:256]
        self.ident_b = self.cb[:, 0:128]
        self.ones_b = self.cb[:, 128:256]
        self.triu_b = self.cb[:, 256:384]

    def phase_mod(self, l):
        S, nc = self.S, self.nc
        with ExitStack() as es:
            modrow = self.sb(es, "modrow", [2, 6 * D])
            bc = self.sb(es, "bc_sb", [128, NSEQ, 6, D])
            wbuf = [self.sb(es, f"modw{i}", [128, 8, 512]) for i in range(2)]
            bbuf = [self.sb(es, f"modb{i}", [1, 512]) for i in range(2)]
            ng = [self.sb(es, f"ng{i}", [128, D]) for i in range(2)]
            psm = [self.ps(es, f"psm{i}", [128, 512]) for i in range(2)]
            psb = [self.ps(es, f"psb{i}", [128, 512]) for i in range(2)]
            S.dma('sp', lambda e: e.dma_start(out=ng[0][:], in_=self.norm1_g[l:l + 1, :].partition_broadcast(128)), w=[ng[0]])
            S.dma('sp', lambda e: e.dma_start(out=ng[1][:], in_=self.norm2_g[l:l + 1, :].partition_broadcast(128)), w=[ng[1]])
            mw = self.mod_w[l].rearrange("(k p) n -> p k n", p=128)
            for j in range(12):
                wb, bb, pm = wbuf[j % 2], bbuf[j % 2], psm[j % 2]
                S.dma('sp', lambda e: e.dma_start(out=wb[:], in_=mw[:, :, j * 512:(j + 1) * 512]), w=[wb])
                S.dma('sp', lambda e: e.dma_start(out=bb[:], in_=self.mod_b[l:l + 1, j * 512:(j + 1) * 512]), w=[bb])
                for k in range(8):
                    S.op('pe', lambda e: e.matmul(pm[0:2, :], lhsT=self.condT[:, k, :], rhs=wb[:, k, :],
                                                  start=(k == 0), stop=False),
                         r=[self.condT, wb], w=[pm])
                S.op('pe', lambda e: e.matmul(pm[0:2, :], lhsT=self.ones_f[0:1, 0:2], rhs=bb[0:1, :],
                                              start=False, stop=True), r=[self.cf, bb], w=[pm])
                S.op('act', lambda e: e.copy(out=modrow[0:2, j * 512:(j + 1) * 512], in_=pm[0:2, :]),
                     r=[pm], w=[modrow])
            n = 0
            for b in range(NSEQ):
                sel = self.cf[0:2, 640 + 128 * b:768 + 128 * b]
                for kind in range(6):
                    for half in range(2):
                        pb = psb[n % 2]
                        n += 1
                        c0 = kind * D + half * 512
                        S.op('pe', lambda e: e.matmul(pb[:], lhsT=sel, rhs=modrow[0:2, c0:c0 + 512],
                                                      start=True, stop=True), r=[self.cf, modrow], w=[pb])
                        dst = bc[:, b, kind, half * 512:(half + 1) * 512]
                        if kind in (1, 4):
                            g = ng[0 if kind == 1 else 1]
                            S.op('dve', lambda e: e.scalar_tensor_tensor(
                                out=dst, in0=pb[:], scalar=1.0, in1=g[:, half * 512:(half + 1) * 512],
                                op0=ALU.add, op1=ALU.mult), r=[pb, g], w=[bc])
                        else:
                            S.op('act', lambda e: e.copy(out=dst, in_=pb[:]), r=[pb], w=[bc])
            for b in range(NSEQ):
                S.dma('sp', lambda e: e.dma_start(out=self.bcD[:, b, :, :], in_=bc[:, b, :, :]), r=[bc], w=[('bcD', b)])
        S.barrier()

    def rms_rstd(self, xin, junk, ss, rstd, n):
        S = self.S
        S.op('act', lambda e: e.activation(out=junk[:], in_=xin[:], func=AF.Square, accum_out=ss[:, 0:1]),
             r=[xin], w=[junk, ss])
        S.op('dve', lambda e: e.tensor_scalar(out=ss[:, 0:1], in0=ss[:, 0:1], scalar1=1.0 / n, scalar2=EPS,
                                              op0=ALU.mult, op1=ALU.add), r=[ss], w=[ss])
        S.op('act', lambda e: e.sqrt(out=ss[:, 0:1], in_=ss[:, 0:1]), r=[ss], w=[ss])
        S.op('dve', lambda e: e.reciprocal(out=rstd[:, 0:1], in_=ss[:, 0:1]), r=[ss], w=[rstd])

    def phase_front(self, l):
        S, nc = self.S, self.nc
        xsrc = self.x if l == 0 else self.xres
        chunks = [(0, 128), (128, 128), (256, 128), (384, 128), (512, 128), (640, 128), (768, 128),
                  (896, 128), (1024, 128), (1152, 128), (1280, 128), (1408, 64), (1472, 8),
                  (1480, 128), (1608, 128)]
        NG = NT // 4
        with ExitStack() as es:
            win = self.sb(es, "win", [128, 8, N_IN], BF16)
            bcl = self.sb(es, "bcl", [128, NSEQ, 2, D])
            for b_ in range(NSEQ):
                S.dma('sp', lambda e: e.dma_start(out=bcl[:, b_, :, :], in_=self.bcD[:, b_, 0:2, :]), w=[bcl])
            xin = [self.sb(es, f"xin{i}", [128, D]) for i in range(4)]
            junk = self.sb(es, "junk", [128, D], BF16)
            hf = [self.sb(es, f"hf{i}", [128, D]) for i in range(2)]
            hb = [self.sb(es, f"hb{i}", [128, D], BF16) for i in range(4)]
            hT = [self.sb(es, f"hT{i}", [128, 8, 512], BF16) for i in range(2)]
            ss = [self.sb(es, f"ss{i}", [128, 4]) for i in range(2)]
            zf = [self.sb(es, f"zf{i}", [128, 512]) for i in range(10)]
            zb = [self.sb(es, f"zb{i}", [128, 512], BF16) for i in range(5)]
            wiT = self.sb(es, "wiT", [8, 512])
            wi_tm = self.sb(es, "wi_tm", [128, 4, 8])
            tp = [self.ps(es, f"tp{i}", [128, D], BF16) for i in range(2)]
            pz = [self.ps(es, f"pz{i}", [128, 512]) for i in range(4)]
            pw = self.ps(es, "pw", [128, 4, 8])
            S.dma('pool', lambda e: e.dma_start(out=win[:], in_=self.w_in[l].rearrange("(k p) n -> p k n", p=128)),
                  w=[win])
            cnt = {"zf": 0, "zb": 0, "pz": 0, "tp": 0}

            def loads(g):
                for t in range(4):
                    i = g * 4 + t
                    S.dma('sp', lambda e: e.dma_start(out=xin[t][:], in_=xsrc[i * 128:(i + 1) * 128, :]), w=[xin[t]])

            def norm(g):
                b = g // (NG // NSEQ)
                s_ = ss[g % 2]
                for t in range(4):
                    S.op('act', lambda e: e.activation(out=junk[:], in_=xin[t][:], func=AF.Square,
                                                       accum_out=s_[:, t:t + 1]), r=[xin[t]], w=[junk, (s_.name, t)])
                keys = [(s_.name, t) for t in range(4)]
                S.op('dve', lambda e: e.tensor_scalar(out=s_[:], in0=s_[:], scalar1=1.0 / D, scalar2=EPS,
                                                      op0=ALU.mult, op1=ALU.add), r=keys, w=keys)
                S.op('act', lambda e: e.sqrt(out=s_[:], in_=s_[:]), r=keys, w=keys)
                S.op('dve', lambda e: e.reciprocal(out=s_[:], in_=s_[:]), r=keys, w=keys)
                for t in range(4):
                    h_ = hf[t % 2]
                    S.op('dve', lambda e: e.scalar_tensor_tensor(out=h_[:], in0=xin[t][:], scalar=s_[:, t:t + 1],
                                                                 in1=bcl[:, b, 1, :], op0=ALU.mult, op1=ALU.mult),
                         r=[xin[t], (s_.name, t), bcl], w=[h_])
                    S.op('pool', lambda e: e.tensor_tensor(out=hb[t][:], in0=h_[:], in1=bcl[:, b, 0, :], op=ALU.add),
                         r=[h_, bcl], w=[hb[t]])

            def trans(g):
                hTg = hT[g % 2]
                for t in range(4):
                    tpi = tp[cnt["tp"] % 2]
                    cnt["tp"] += 1
                    for k in range(8):
                        S.op('pe', lambda e: e.transpose(tpi[:, k * 128:(k + 1) * 128], hb[t][:, k * 128:(k + 1) * 128],
                                                         self.ident_b), r=[hb[t], self.cb], w=[tpi])
                    S.op('act', lambda e: e.copy(out=hTg[:, :, t * 128:(t + 1) * 128],
                                                 in_=tpi[:].rearrange("p (k t) -> p k t", k=8)), r=[tpi],
                         w=[(hTg.name, t)])

            def proj(g):
                stores = []
                hTg = hT[g % 2]
                hkeys = [(hTg.name, t) for t in range(4)]
                b = g // (NG // NSEQ)
                gs = g % (NG // NSEQ)
                cs = slice(gs * 512, (gs + 1) * 512)
                for ci, (c0, m) in enumerate(chunks):
                    p = pz[cnt["pz"] % 4]
                    cnt["pz"] += 1
                    for k in range(8):
                        S.op('pe', lambda e: e.matmul(p[0:m, :], lhsT=win[:, k, c0:c0 + m], rhs=hTg[:, k, :],
                                                      start=(k == 0), stop=(k == 7)), r=[win] + hkeys, w=[p])
                    if 7 <= ci <= 11:
                        z = zb[cnt["zb"] % 5]
                        cnt["zb"] += 1
                        S.op('act', lambda e: e.copy(out=z[0:m, :], in_=p[0:m, :]), r=[p], w=[z])
                        dst = self.zQI[b, (ci - 7) * 128:(ci - 6) * 128, cs] if ci < 11 else self.zKI[b, :, cs]
                        stores.append(lambda dst=dst, z=z, m=m, ci=ci: S.dma(
                            'sp', lambda e: e.dma_start(out=dst, in_=z[0:m, :]), r=[z], w=[("z", ci, g)]))
                    elif ci == 12:
                        S.op('act', lambda e: e.copy(out=wiT[:], in_=p[0:8, :]), r=[p], w=[wiT])
                        for s4 in range(4):
                            S.op('pe', lambda e: e.transpose(pw[:, s4, :], wiT[0:8, s4 * 128:(s4 + 1) * 128],
                                                             self.ident_f[0:8, 0:8]), r=[wiT, self.cf], w=[pw])
                        S.op('act', lambda e: e.copy(out=wi_tm[:], in_=pw[:]), r=[pw], w=[wi_tm])
                        stores.append(lambda: S.dma('sp', lambda e: e.dma_start(
                            out=self.zWI[b, cs, :].rearrange("(s p) h -> p s h", p=128), in_=wi_tm[:]),
                            r=[wi_tm], w=[("z", 12, g)]))
                    else:
                        z = zf[cnt["zf"] % 10]
                        cnt["zf"] += 1
                        S.op('dve', lambda e: e.tensor_copy(out=z[0:m, :], in_=p[0:m, :]), r=[p], w=[z])
                        if ci < 4:
                            dst = self.zA[b, ci * 128:(ci + 1) * 128, cs]
                        elif ci < 6:
                            dst = self.zCQ[b, (ci - 4) * 128:(ci - 3) * 128, cs]
                        elif ci == 6:
                            dst = self.zCKV[b, :, cs]
                        else:
                            dst = self.zUP[b, (ci - 13) * 128:(ci - 12) * 128, cs]
                        stores.append(lambda dst=dst, z=z, m=m, ci=ci: S.dma(
                            'sp', lambda e: e.dma_start(out=dst, in_=z[0:m, :]), r=[z], w=[("z", ci, g)]))
                return stores

            loads(0)
            norm(0)
            trans(0)
            pending = []
            for g in range(NG):
                if g + 1 < NG:
                    loads(g + 1)
                for fn in pending:
                    fn()
                if g + 1 < NG:
                    norm(g + 1)
                pending = proj(g)
                if g + 1 < NG:
                    trans(g + 1)
            for fn in pending:
                fn()
        S.barrier()

    def phase_conv(self, l):
        S, nc = self.S, self.nc
        with ExitStack() as es:
            ck = self.sb(es, "ck", [128, 2, 31])
            cv = self.sb(es, "cv", [128, 3, 2])
            dg = self.sb(es, "cdg", [128, 2, 31, 128], BF16)
            av = [self.sb(es, f"av{i}", [128, SEQ]) for i in range(2)]
            ag = [self.sb(es, f"ag{i}", [128, SEQ]) for i in range(2)]
            upad = [self.sb(es, f"upad{i}", [128, 30 + SEQ], BF16) for i in range(2)]
            acc = [self.sb(es, f"cacc{i}", [128, 512]) for i in range(2)]
            sq = [self.sb(es, f"csq{i}", [128, 512]) for i in range(2)]
            mean = self.sb(es, "cmean", [128, 512])
            var = self.sb(es, "cvar", [128, 512])
            t1 = [self.sb(es, f"ct1{i}", [128, 512]) for i in range(2)]
            yb = [self.sb(es, f"cyb{i}", [128, 512], BF16) for i in range(4)]
            pcv = [self.ps(es, f"cpc{i}", [128, 512]) for i in range(4)]
            pss = self.ps(es, "cps_s", [128, 512])
            psq = self.ps(es, "cps_q", [128, 512])
            with nc.allow_non_contiguous_dma(reason="tiny transposed parameter loads"):
                for ch in range(2):
                    S.dma('sp', lambda e: e.dma_start(
                        out=ck[:, ch, :], in_=self.conv_k[l][:, ch * 128:(ch + 1) * 128].rearrange("k p -> p k")), w=[ck])
                for j, src in enumerate((self.conv_b, self.conv_ln_g, self.conv_ln_b)):
                    S.dma('sp', lambda e: e.dma_start(out=cv[:, j, :], in_=src[l].rearrange("(c p) -> p c", p=128)),
                          w=[cv])
            for ch in range(2):
                S.op('pool', lambda e: e.tensor_tensor(
                    out=dg[:, ch, :, :], in0=self.ident_b.unsqueeze(1).to_broadcast([128, 31, 128]),
                    in1=ck[:, ch, :].unsqueeze(2).to_broadcast([128, 31, 128]), op=ALU.mult),
                    r=[self.cb, ck], w=[dg])
            for i in range(2):
                S.op('pool', lambda e: e.memset(upad[i][:, 0:30], 0.0), w=[upad[i]])
            cepsb = self.sb(es, "ceps", [128, 1])
            S.op('pool', lambda e: e.memset(cepsb[:], EPS), w=[cepsb])
            ny = 0
            npc = 0
            for b in range(NSEQ):
                for ch in range(2):
                    S.dma('sp', lambda e: e.dma_start(out=av[ch][:], in_=self.zA[b, ch * 128:(ch + 1) * 128, :]),
                          w=[av[ch]])
                    S.dma('sp', lambda e: e.dma_start(out=ag[ch][:],
                                                      in_=self.zA[b, 256 + ch * 128:256 + (ch + 1) * 128, :]),
                          w=[ag[ch]])
                    S.op('act', lambda e: e.activation(out=ag[ch][:], in_=ag[ch][:], func=AF.Sigmoid), r=[ag[ch]],
                         w=[ag[ch]])
                    S.op('dve' if ch == 0 else 'pool',
                         lambda e: e.tensor_tensor(out=upad[ch][:, 30:], in0=av[ch][:], in1=ag[ch][:], op=ALU.mult),
                         r=[av[ch], ag[ch]], w=[upad[ch]])
                for tt in range(4):
                    cs = slice(tt * 512, (tt + 1) * 512)
                    for ch in range(2):
                        p = pcv[npc % 4]
                        npc += 1
                        for k in range(31):
                            S.op('pe', lambda e: e.matmul(p[:], lhsT=dg[:, ch, k, :],
                                                          rhs=upad[ch][:, tt * 512 + k:tt * 512 + k + 512],
                                                          start=(k == 0), stop=(k == 30)), r=[dg, upad[ch]], w=[p])
                        S.op('act', lambda e: e.activation(out=acc[ch][:], in_=p[:], func=AF.Identity,
                                                           bias=cv[:, 0, ch:ch + 1], scale=1.0), r=[p, cv], w=[acc[ch]])
                        S.op('act', lambda e: e.activation(out=sq[ch][:], in_=acc[ch][:], func=AF.Square),
                             r=[acc[ch]], w=[sq[ch]])
                    for ch in range(2):
                        S.op('pe', lambda e: e.matmul(pss[:], lhsT=self.ones_f, rhs=acc[ch][:], start=(ch == 0),
                                                      stop=(ch == 1)), r=[acc[ch], self.cf], w=[pss])
                    for ch in range(2):
                        S.op('pe', lambda e: e.matmul(psq[:], lhsT=self.ones_f, rhs=sq[ch][:], start=(ch == 0),
                                                      stop=(ch == 1)), r=[sq[ch], self.cf], w=[psq])
                    S.op('act', lambda e: e.mul(out=mean[:], in_=pss[:], mul=1.0 / 256), r=[pss], w=[mean])
                    S.op('dve', lambda e: e.tensor_tensor(out=var[:], in0=mean[:], in1=mean[:], op=ALU.mult),
                         r=[mean], w=[var])
                    S.op('dve', lambda e: e.scalar_tensor_tensor(out=var[:], in0=psq[:], scalar=1.0 / 256, in1=var[:],
                                                                 op0=ALU.mult, op1=ALU.subtract), r=[psq, var], w=[var])
                    S.op('act', lambda e: e.activation(out=var[:], in_=var[:], func=AF.Ln, bias=cepsb[:, 0:1], scale=1.0),
                         r=[var, cepsb], w=[var])
                    S.op('act', lambda e: e.activation(out=var[:], in_=var[:], func=AF.Exp, scale=-0.5), r=[var], w=[var])
                    for ch in range(2):
                        y = yb[ny % 4]
                        ny += 1
                        S.op('dve', lambda e: e.tensor_tensor(out=t1[ch][:], in0=acc[ch][:], in1=mean[:],
                                                              op=ALU.subtract), r=[acc[ch], mean], w=[t1[ch]])
                        S.op('dve', lambda e: e.tensor_tensor(out=t1[ch][:], in0=t1[ch][:], in1=var[:], op=ALU.mult),
                             r=[t1[ch], var], w=[t1[ch]])
                        S.op('act', lambda e: e.activation(out=y[:], in_=t1[ch][:], func=AF.Silu,
                                                           bias=cv[:, 2, ch:ch + 1], scale=cv[:, 1, ch:ch + 1]),
                             r=[t1[ch], cv], w=[y])
                        S.dma('pool', lambda e: e.dma_start(out=self.mixT[b, ch * 128:(ch + 1) * 128, cs], in_=y[:]),
                              r=[y], w=[("mixT", b, ch, tt)])
        S.barrier()

    def phase_pool(self, l):
        S, nc = self.S, self.nc
        with ExitStack() as es:
            pinv = self.sb(es, "pinv_sb", [128, 2, SEQ])
            bdf = self.sb(es, "bdf", [128, 2, 128])
            bd = self.sb(es, "bd", [128, 2, 128], BF16)
            psc = self.sb(es, "psc", [128, 2])
            P = [self.sb(es, f"pp{i}", [128, 16 + SEQ]) for i in range(5)]
            dd = self.sb(es, "pd", [128, SEQ])
            db = self.sb(es, "pdb", [128, SEQ], BF16)
            yb = [self.sb(es, f"pyb{i}", [128, 512], BF16) for i in range(2)]
            pp = [self.ps(es, f"pps{i}", [128, 512]) for i in range(2)]
            S.dma('sp', lambda e: e.dma_start(out=pinv[:], in_=self.pinv_d[:, :, :]), w=[pinv])
            S.op('pool', lambda e: e.memset(bdf[:], 0.0), w=[bdf])
            for g in range(4):
                ch, half = divmod(g, 2)
                S.dma('sp', lambda e: e.dma_start(out=bdf[half * 64:(half + 1) * 64, ch, half * 64:(half + 1) * 64],
                                                  in_=self.pool_w[l, g]), w=[bdf])
            S.op('act', lambda e: e.copy(out=bd[:], in_=bdf[:]), r=[bdf], w=[bd])
            with nc.allow_non_contiguous_dma(reason="tiny transposed parameter load"):
                S.dma('sp', lambda e: e.dma_start(out=psc[:], in_=self.pool_scale[l].rearrange("(c p) -> p c", p=128)),
                      w=[psc])
            for i in range(5):
                S.op('pool', lambda e: e.memset(P[i][:, 0:16], 0.0), w=[P[i]])
            n = 0
            for b in range(NSEQ):
                for ch in range(2):
                    S.dma('sp', lambda e: e.dma_start(out=P[0][:, 16:], in_=self.zUP[b, ch * 128:(ch + 1) * 128, :]),
                          w=[P[0]])
                    for j, sh in enumerate((1, 2, 4, 8)):
                        if ch == 0 and j >= 2:
                            break
                        eng = 'dve'
                        S.op(eng, lambda e: e.tensor_tensor(out=P[j + 1][:, 16:], in0=P[j][:, 16:],
                                                            in1=P[j][:, 16 - sh:16 - sh + SEQ], op=ALU.add),
                             r=[P[j]], w=[P[j + 1]])
                    lo_src, hi_src = (P[1], P[2]) if ch == 0 else (P[3], P[4])
                    for half, src in ((0, lo_src), (1, hi_src)):
                        ps_ = slice(half * 64, (half + 1) * 64)
                        S.op('dve', lambda e: e.tensor_tensor(out=dd[ps_, :], in0=src[ps_, 16:], in1=pinv[ps_, ch, :],
                                                              op=ALU.mult), r=[src, pinv], w=[dd])
                    S.op('dve', lambda e: e.tensor_tensor(out=db[:], in0=dd[:], in1=P[0][:, 16:], op=ALU.subtract),
                         r=[dd, P[0]], w=[db])
                    for tt in range(4):
                        cs = slice(tt * 512, (tt + 1) * 512)
                        p = pp[n % 2]
                        y = yb[n % 2]
                        n += 1
                        S.op('pe', lambda e: e.matmul(p[:], lhsT=bd[:, ch, :], rhs=db[:, cs], start=True, stop=True),
                             r=[bd, db], w=[p])
                        S.op('act', lambda e: e.mul(out=y[:], in_=p[:], mul=psc[:, ch:ch + 1]), r=[p, psc], w=[y])
                        S.dma('sp', lambda e: e.dma_start(out=self.mixT[b, 768 + ch * 128:768 + (ch + 1) * 128, cs],
                                                          in_=y[:]), r=[y], w=[("mixT", b, 6 + ch, tt)])
        S.barrier()

    def phase_attn(self, l, nit=12):
        S, nc = self.S, self.nc
        NQB = SEQ // 128
        with ExitStack() as es:
            pb = [self.ps(es, f"ab{i}", [128, 512]) for i in range(8)]
            wuq = self.sb(es, "wuq", [128, 2, 512], BF16)
            wukf = self.sb(es, "wukf", [128, 512])
            wukT = self.sb(es, "wukT", [128, 4, 128], BF16)
            wuvf = self.sb(es, "wuvf", [128, 8, 128])
            wuvp = self.sb(es, "wuvp", [128, 8, 128], BF16)
            gq = self.sb(es, "gq", [128, 2])
            gkv = self.sb(es, "gkv", [128, 1])
            cqn = self.sb(es, "cqn", [128, 2, SEQ], BF16)
            ckvT = self.sb(es, "ckvT", [128, SEQ], BF16)
            vtok = self.sb(es, "vtok", [128, NQB, 128], BF16)
            qabs = self.sb(es, "qabs", [128, 8, SEQ], BF16)
            qiT = self.sb(es, "qiT", [128, 4, SEQ], BF16)
            ki2 = self.sb(es, "ki2", [128, SEQ], BF16)
            wi = self.sb(es, "wi", [128, NQB, 8])
            wab = self.sb(es, "wab", [128, NQB, 8])
            wsg = self.sb(es, "wsg", [128, NQB, 8])
            ctxn = self.sb(es, "ctxn", [128, 8, SEQ], BF16)
            qT = ctxn
            ld = [self.sb(es, f"ald{i}", [128, SEQ]) for i in range(2)]
            sqs = [self.sb(es, f"asq{i}", [128, 512]) for i in range(4)]
            rss = [self.sb(es, f"ars{i}", [128, 512]) for i in range(2)]
            dsg = self.sb(es, "dsg", [128, 8, 128], BF16)
            term = [self.sb(es, f"term{i}", [128, 512], BF16) for i in range(3)]
            scs = ld
            junk = self.sb(es, "ajunk", [128, SEQ], BF16)
            masks = [self.sb(es, f"mask{i}", [128, SEQ], BF16) for i in range(2)]
            maskT = [self.sb(es, f"maskT{i}", [128, NQB, 128], BF16) for i in range(2)]
            PT = [self.sb(es, f"PT{i}", [128, 512], BF16) for i in range(4)]
            PTm = [self.sb(es, f"PTm{i}", [128, 512], BF16) for i in range(2)]
            rden = self.sb(es, "rden", [128, 1024])
            st = self.sb(es, "ast", [128, 8])
            nbias = self.sb(es, "anb", [128, 1])
            epsb = self.sb(es, "aeps", [128, 1])
            S.op('pool', lambda e: e.memset(epsb[:], EPS), w=[epsb])
            S.op('pool', lambda e: e.memset(nbias[:], -30000.0), w=[nbias])
            W = self.sb(es, "aW", [128, nit + 1])
            W2 = self.sb(es, "aW2", [128, nit + 1])
            ob = [self.sb(es, f"aob{i}", [128, 512], BF16) for i in range(2)]
            S.dma('pool', lambda e: e.dma_start(out=wuq[:], in_=self.w_uq[l].rearrange("(k p) n -> p k n", p=128)),
                  w=[wuq])
            S.dma('sp', lambda e: e.dma_start(out=wukf[:], in_=self.w_uk[l]), w=[wukf])
            pv = pb[0][:].rearrange("p (j k) -> p j k", j=4)
            for j in range(4):
                S.op('pe', lambda e: e.transpose(pv[:, j, :], wukf[:, j * 128:(j + 1) * 128], self.ident_f),
                     r=[wukf, self.cf], w=[pb[0]])
            S.op('act', lambda e: e.copy(out=wukT[:], in_=pv), r=[pb[0]], w=[wukT])
            S.op('pool', lambda e: e.memset(wuvf[:], 0.0), w=[wuvf])
            wv = self.w_uv[l].rearrange("k (j two d) -> k j two d", two=2, d=64)
            wvf = wuvf[:].rearrange("p (j two) c -> p j two c", two=2)
            for par in range(2):
                S.dma('sp', lambda e: e.dma_start(out=wvf[:, :, par, par * 64:(par + 1) * 64], in_=wv[:, :, par, :]),
                      w=[wuvf])
            S.op('act', lambda e: e.copy(out=wuvp[:], in_=wuvf[:]), r=[wuvf], w=[wuvp])
            with nc.allow_non_contiguous_dma(reason="tiny transposed parameter loads"):
                S.dma('sp', lambda e: e.dma_start(out=gq[:], in_=self.q_norm_g[l].rearrange("(c p) -> p c", p=128)),
                      w=[gq])
                S.dma('sp', lambda e: e.dma_start(out=gkv[:], in_=self.kv_norm_g[l].rearrange("(c p) -> p c", p=128)),
                      w=[gkv])
            for b in range(NSEQ):
                for ch in range(2):
                    S.dma('sp', lambda e: e.dma_start(out=ld[ch][:], in_=self.zCQ[b, ch * 128:(ch + 1) * 128, :]),
                          w=[ld[ch]])
                for tt in range(4):
                    cs = slice(tt * 512, (tt + 1) * 512)
                    rs = rss[tt % 2]
                    pn = pb[tt % 2]
                    for ch in range(2):
                        sq = sqs[(tt % 2) * 2 + ch]
                        S.op('act', lambda e: e.activation(out=sq[:], in_=ld[ch][:, cs], func=AF.Square),
                             r=[ld[ch]], w=[sq])
                        S.op('pe', lambda e: e.matmul(pn[:], lhsT=self.ones_f, rhs=sq[:], start=(ch == 0),
                                                      stop=(ch == 1)), r=[sq, self.cf], w=[pn])
                    S.op('act', lambda e: e.activation(out=rs[:], in_=pn[:], func=AF.Ln, bias=epsb[:, 0:1],
                                                       scale=1.0 / 256), r=[pn, epsb], w=[rs])
                    S.op('act', lambda e: e.activation(out=rs[:], in_=rs[:], func=AF.Exp, scale=-0.5), r=[rs], w=[rs])
                    for ch in range(2):
                        S.op('dve', lambda e: e.scalar_tensor_tensor(out=cqn[:, ch, cs], in0=ld[ch][:, cs],
                                                                     scalar=gq[:, ch:ch + 1], in1=rs[:],
                                                                     op0=ALU.mult, op1=ALU.mult),
                             r=[ld[ch], gq, rs], w=[cqn])
                S.dma('sp', lambda e: e.dma_start(out=ld[0][:], in_=self.zCKV[b, :, :]), w=[ld[0]])
                for tt in range(4):
                    cs = slice(tt * 512, (tt + 1) * 512)
                    rs = rss[tt % 2]
                    sq = sqs[tt % 4]
                    pn = pb[tt % 2]
                    S.op('act', lambda e: e.activation(out=sq[:], in_=ld[0][:, cs], func=AF.Square), r=[ld[0]], w=[sq])
                    S.op('pe', lambda e: e.matmul(pn[:], lhsT=self.ones_f, rhs=sq[:], start=True, stop=True),
                         r=[sq, self.cf], w=[pn])
                    S.op('act', lambda e: e.activation(out=rs[:], in_=pn[:], func=AF.Ln, bias=epsb[:, 0:1],
                                                       scale=1.0 / 128), r=[pn, epsb], w=[rs])
                    S.op('act', lambda e: e.activation(out=rs[:], in_=rs[:], func=AF.Exp, scale=-0.5), r=[rs], w=[rs])
                    S.op('dve', lambda e: e.scalar_tensor_tensor(out=ckvT[:, cs], in0=ld[0][:, cs], scalar=gkv[:, 0:1],
                                                                 in1=rs[:], op0=ALU.mult, op1=ALU.mult),
                         r=[ld[0], gkv, rs], w=[ckvT])
                for half in range(2):
                    tpv = pb[2 + half][:].bitcast(BF16).rearrange("p (j k) -> p j k", j=8)
                    for j in range(8):
                        sci = half * 8 + j
                        S.op('pe', lambda e: e.transpose(tpv[:, j, :], ckvT[:, sci * 128:(sci + 1) * 128], self.ident_b),
                             r=[ckvT, self.cb], w=[pb[2 + half]])
                    S.op('act', lambda e: e.copy(out=vtok[:, half * 8:(half + 1) * 8, :], in_=tpv),
                         r=[pb[2 + half]], w=[vtok])
                n = 0
                for m in range(4):
                    for tt in range(4):
                        cs = slice(tt * 512, (tt + 1) * 512)
                        p = pb[4 + n % 2]
                        n += 1
                        for k in range(2):
                            S.op('pe', lambda e: e.matmul(p[:], lhsT=wuq[:, k, m * 128:(m + 1) * 128], rhs=cqn[:, k, cs],
                                                          start=(k == 0), stop=(k == 1)), r=[wuq, cqn], w=[p])
                        if n % 2 == 0:
                            S.op('act', lambda e: e.copy(out=qT[:, m, cs], in_=p[:]), r=[p], w=[qT])
                        else:
                            S.op('dve', lambda e: e.tensor_copy(out=qT[:, m, cs], in_=p[:]), r=[p], w=[qT])
                for h in range(8):
                    hp = slice((h % 2) * 64, (h % 2) * 64 + 64)
                    for tt in range(4):
                        cs = slice(tt * 512, (tt + 1) * 512)
                        p = pb[4 + n % 2]
                        n += 1
                        S.op('pe', lambda e: e.matmul(p[:], lhsT=wukT[hp, h // 2, :], rhs=qT[hp, h // 2, cs],
                                                      start=True, stop=True), r=[wukT, qT], w=[p])
                        if n % 2 == 0:
                            S.op('act', lambda e: e.mul(out=qabs[:, h, cs], in_=p[:], mul=0.125), r=[p], w=[qabs])
                        else:
                            S.op('dve', lambda e: e.tensor_scalar(out=qabs[:, h, cs], in0=p[:], scalar1=0.125,
                                                                  scalar2=None, op0=ALU.mult), r=[p], w=[qabs])
                S.dma('sp', lambda e: e.dma_start(out=qiT[:], in_=self.zQI[b].rearrange("(m p) t -> p m t", p=128)),
                      w=[qiT])
                for half in range(2):
                    S.dma('sp', lambda e: e.dma_start(out=ki2[half * 64:(half + 1) * 64, :], in_=self.zKI[b, :, :]),
                          w=[ki2])
                S.dma('sp', lambda e: e.dma_start(out=wi[:], in_=self.zWI[b].rearrange("(q p) h -> p q h", p=128)),
                      w=[wi])
                S.op('act', lambda e: e.activation(out=wab[:], in_=wi[:], func=AF.Abs, scale=float(512 ** -0.5)),
                     r=[wi], w=[wab])
                S.op('act', lambda e: e.activation(out=wsg[:], in_=wi[:], func=AF.Sign), r=[wi], w=[wsg])
                def indexer(qb):
                    sc = scs[qb % 2]
                    nk = (qb + 1) * 128
                    nkt = (nk + 511) // 512
                    qs = slice(qb * 128, (qb + 1) * 128)
                    S.op('pool', lambda e: e.tensor_tensor(
                        out=dsg[:], in0=self.ident_b.unsqueeze(1).to_broadcast([128, 8, 128]),
                        in1=wsg[:, qb, :].unsqueeze(2).to_broadcast([128, 8, 128]), op=ALU.mult),
                        r=[self.cb, wsg], w=[dsg])
                    for kt in range(nkt):
                        w_ = min(512, nk - kt * 512)
                        ks = slice(kt * 512, kt * 512 + w_)
                        psc = pb[1]

                        def rel(h):
                            hp = slice((h % 2) * 64, (h % 2) * 64 + 64)
                            p = pb[0]
                            S.op('pe', lambda e: e.matmul(p[:, 0:w_], lhsT=qiT[hp, h // 2, qs], rhs=ki2[hp, ks],
                                                          start=True, stop=True), r=[qiT, ki2], w=[p])
                        rel(0)
                        for h in range(8):
                            p = pb[0]
                            tm = term[h % 3]
                            S.op('act', lambda e: e.activation(out=tm[:, 0:w_], in_=p[:, 0:w_], func=AF.Relu,
                                                               scale=wab[:, qb, h:h + 1]), r=[p, wab], w=[tm])
                            if h + 1 < 8:
                                rel(h + 1)
                            S.op('pe', lambda e: e.matmul(psc[:, 0:w_], lhsT=dsg[:, h, :], rhs=tm[:, 0:w_],
                                                          start=(h == 0), stop=(h == 7)), r=[dsg, tm], w=[psc])
                            if h < 7:
                                yield
                        S.op('act', lambda e: e.copy(out=sc[:, ks], in_=psc[:, 0:w_]), r=[psc], w=[sc])
                        yield
                    S.op('pool', lambda e: e.memset(sc[0:64, nk - 64:nk], -1e30), r=[], w=[sc])

                def bisect(qb):
                    sc = scs[qb % 2]
                    mask = masks[qb % 2]
                    nk = (qb + 1) * 128
                    if qb < 2:
                        S.op('dve', lambda e: e.tensor_scalar(out=mask[:, 0:nk], in0=sc[:, 0:nk], scalar1=-1e29,
                                                              scalar2=None, op0=ALU.is_ge), r=[sc], w=[mask])
                        return
                    S.op('dve', lambda e: e.reduce_max(out=st[:, 0:1], in_=sc[:, 0:nk], axis=AX.X), r=[sc], w=[st])
                    S.op('dve', lambda e: e.tensor_reduce(out=st[:, 2:3], in_=sc[:, 0:nk - 64], axis=AX.X, op=ALU.min),
                         r=[sc], w=[st])
                    S.op('dve', lambda e: e.tensor_tensor(out=st[:, 1:2], in0=st[:, 0:1], in1=st[:, 2:3],
                                                          op=ALU.subtract), r=[st], w=[st])
                    S.op('dve', lambda e: e.tensor_scalar(out=W[:], in0=self.cf[:, 896:896 + nit + 1], scalar1=st[:, 1:2],
                                                          scalar2=None, op0=ALU.mult), r=[st, self.cf], w=[W])
                    S.op('dve', lambda e: e.tensor_scalar(out=W2[:], in0=W[:], scalar1=2.0, scalar2=None, op0=ALU.mult),
                         r=[W], w=[W2])
                    S.op('dve', lambda e: e.tensor_tensor(out=st[:, 3:4], in0=st[:, 2:3], in1=W[:, 0:1], op=ALU.add),
                         r=[st, W], w=[st])
                    for i in range(nit):
                        S.op('dve', lambda e: e.tensor_scalar(out=junk[:, 0:nk], in0=sc[:, 0:nk], scalar1=st[:, 3:4],
                                                              scalar2=0.0, op0=ALU.is_ge, op1=ALU.add,
                                                              accum_out=st[:, 4:5]), r=[sc, st], w=[junk, st])
                        S.op('dve', lambda e: e.tensor_scalar(out=st[:, 5:6], in0=st[:, 4:5], scalar1=float(TOPK),
                                                              scalar2=W2[:, i + 1:i + 2], op0=ALU.is_ge, op1=ALU.mult),
                             r=[st, W2], w=[st])
                        S.op('dve', lambda e: e.scalar_tensor_tensor(out=st[:, 3:4], in0=st[:, 5:6],
                                                                     scalar=W[:, i + 1:i + 2], in1=st[:, 3:4],
                                                                     op0=ALU.subtract, op1=ALU.add), r=[st, W], w=[st])
                    S.op('dve', lambda e: e.tensor_tensor(out=st[:, 3:4], in0=st[:, 3:4], in1=W[:, nit:nit + 1],
                                                          op=ALU.subtract), r=[st, W], w=[st])
                    S.op('dve', lambda e: e.tensor_scalar(out=mask[:, 0:nk], in0=sc[:, 0:nk], scalar1=st[:, 3:4],
                                                          scalar2=None, op0=ALU.is_ge), r=[sc, st], w=[mask])

                def masktr(qb):
                    mT = maskT[qb % 2]
                    mask = masks[qb % 2]
                    for half in range((qb + 8) // 8):
                        tpv = pb[1][:].bitcast(BF16).rearrange("p (j k) -> p j k", j=8)
                        nj = min(8, qb + 1 - half * 8)
                        for j in range(nj):
                            sci = half * 8 + j
                            S.op('pe', lambda e: e.transpose(tpv[:, j, :], mask[:, sci * 128:(sci + 1) * 128],
                                                             self.ident_b), r=[mask, self.cb], w=[pb[1]])
                        S.op('act', lambda e: e.activation(out=mT[:, half * 8:half * 8 + nj, :], in_=tpv[:, 0:nj, :],
                                                           func=AF.Identity, bias=nbias[:, 0:1], scale=30000.0),
                             r=[pb[1], nbias], w=[mT])

                def attend(qb):
                    qs = slice(qb * 128, (qb + 1) * 128)
                    mT = maskT[qb % 2]
                    npt = 0

                    def lgmm(sci, hh):
                        pl = pb[2 + hh]
                        S.op('pe', lambda e: e.matmul(pl[:].rearrange("p (h q) -> p h q", h=4),
                                                      lhsT=ckvT[:, sci * 128:(sci + 1) * 128],
                                                      rhs=qabs[:, hh * 4:(hh + 1) * 4, qs], start=True, stop=False),
                             r=[ckvT, qabs], w=[pl])
                        S.op('pe', lambda e: e.matmul(pl[:].rearrange("p (h q) -> p h q", h=4),
                                                      lhsT=self.ident_b,
                                                      rhs=mT[:, sci, :].unsqueeze(1).to_broadcast([128, 4, 128]),
                                                      start=False, stop=True), r=[self.cb, mT], w=[pl])
                    lgmm(0, 0)
                    lgmm(0, 1)
                    for sci in range(qb + 1):
                        for hh in range(2):
                            pl, pc, pd = pb[2 + hh], pb[4 + hh], pb[6 + hh]
                            pt = PT[npt % 4]
                            npt += 1
                            S.op('act', lambda e: e.activation(out=pt[:, 0:512], in_=pl[:], func=AF.Exp),
                                 r=[pl], w=[pt])
                            if sci + 1 <= qb:
                                lgmm(sci + 1, hh)
                            S.op('pe', lambda e: e.matmul(pc[:], lhsT=vtok[:, sci, :], rhs=pt[:, 0:512],
                                                          start=(sci == 0), stop=(sci == qb)), r=[vtok, pt], w=[pc])
                            S.op('pe', lambda e: e.matmul(pd[:], lhsT=self.ones_b, rhs=pt[:, 0:512],
                                                          start=(sci == 0), stop=(sci == qb)), r=[self.cb, pt], w=[pd])
                            yield
                    for hh in range(2):
                        S.op('act', lambda e: e.activation(out=rden[:, hh * 512:(hh + 1) * 512], in_=pb[6 + hh][:],
                                                           func=AF.Ln), r=[pb[6 + hh]], w=[(rden.name, hh)])
                        S.op('act', lambda e: e.activation(out=rden[:, hh * 512:(hh + 1) * 512],
                                                           in_=rden[:, hh * 512:(hh + 1) * 512], func=AF.Exp, scale=-1.0),
                             r=[(rden.name, hh)], w=[(rden.name, hh)])
                        S.op('dve', lambda e: e.tensor_tensor(
                            out=ctxn[:, hh * 4:(hh + 1) * 4, qs],
                            in0=pb[4 + hh][:].rearrange("p (h q) -> p h q", h=4),
                            in1=rden[:, hh * 512:(hh + 1) * 512].rearrange("p (h q) -> p h q", h=4), op=ALU.mult),
                            r=[pb[4 + hh], (rden.name, hh)], w=[ctxn])

                def drain(g):
                    for _ in g:
                        pass

                def interleave(ga, gb):
                    ga, gb = iter(ga), iter(gb)
                    da = db = False
                    while not (da and db):
                        if not da:
                            try:
                                next(ga)
                            except StopIteration:
                                da = True
                        if not db:
                            try:
                                next(gb)
                            except StopIteration:
                                db = True

                drain(indexer(0))
                bisect(0)
                drain(indexer(1))
                for qb in range(NQB):
                    if qb + 1 < NQB:
                        bisect(qb + 1)
                    masktr(qb)
                    interleave(attend(qb), indexer(qb + 2) if qb + 2 < NQB else iter(()))

                n = 0
                for j in range(4):
                    for tt in range(4):
                        cs = slice(tt * 512, (tt + 1) * 512)
                        p = pb[n % 2]
                        o = ob[n % 2]
                        n += 1
                        for par in range(2):
                            S.op('pe', lambda e: e.matmul(p[:], lhsT=wuvp[:, 2 * j + par, :], rhs=ctxn[:, 2 * j + par, cs],
                                                          start=(par == 0), stop=(par == 1)), r=[wuvp, ctxn], w=[p])
                        S.op('act', lambda e: e.copy(out=o[:], in_=p[:]), r=[p], w=[o])
                        S.dma('sp', lambda e: e.dma_start(out=self.mixT[b, 256 + j * 128:256 + (j + 1) * 128, cs],
                                                          in_=o[:]), r=[o], w=[("mixT", b, 2 + j, tt)])
        S.barrier()

    def phase_wout(self, l):
        S, nc = self.S, self.nc
        xsrc = self.x if l == 0 else self.xres
        with ExitStack() as es:
            pb = [self.ps(es, f"wb{i}", [128, 512]) for i in range(8)]
            wout = self.sb(es, "wout", [128, 8, D], BF16)
            bcl = self.sb(es, "bcl2", [128, NSEQ, 3, D])
            rw = self.sb(es, "rw", [128, 8, 36])
            rb = self.sb(es, "rb", [1, 36])
            mx = [self.sb(es, f"mx{i}", [128, 8, 512], BF16) for i in range(2)]
            xin = [self.sb(es, f"wx{i}", [128, D]) for i in range(3)]
            x1 = [self.sb(es, f"wx1{i}", [128, D]) for i in range(2)]
            tmp = self.sb(es, "wtmp", [128, D])
            junk = self.sb(es, "wjunk", [128, D], BF16)
            h2 = self.sb(es, "wh2", [128, D])
            h2b = [self.sb(es, f"wh2b{i}", [128, D], BF16) for i in range(2)]
            h2T = self.sb(es, "wh2T", [128, 8, 128])
            ss = self.sb(es, "wss", [128, 1])
            rstd = self.sb(es, "wrstd", [128, 1])
            RL = self.sb(es, "RL", [128, NT, 36])
            S.dma('pool', lambda e: e.dma_start(out=wout[:], in_=self.w_out[l].rearrange("(k p) n -> p k n", p=128)),
                  w=[wout])
            for b in range(NSEQ):
                S.dma('sp', lambda e: e.dma_start(out=bcl[:, b, :, :], in_=self.bcD[:, b, 2:5, :]), w=[bcl])
            S.dma('sp', lambda e: e.dma_start(out=rw[:], in_=self.router_w[l].rearrange("(k p) n -> p k n", p=128)),
                  w=[rw])
            S.dma('sp', lambda e: e.dma_start(out=rb[:], in_=self.router_b[l:l + 1, :]), w=[rb])
            zt = self.sb(es, "hz", [128, 5, D], BF16)
            S.op('pool', lambda e: e.memset(zt[:], 0.0), w=[zt])
            hsv = self.Hs.rearrange("(n p) d -> p n d", p=128)
            zkeys = []
            for j in range(PSLOT // 128 // 5):
                zkeys.append(("Hs0", j))
                S.dma('act', lambda e: e.dma_start(out=hsv[:, j * 5:(j + 1) * 5, :], in_=zt[:]), r=[zt], w=[zkeys[-1]])

            def load_mx(g):
                b_, t0 = g // 4, (g % 4) * 512
                for k in range(8):
                    S.dma('sp', lambda e: e.dma_start(out=mx[g % 2][:, k, :],
                                                      in_=self.mixT[b_, k * 128:(k + 1) * 128, t0:t0 + 512]),
                          w=[(mx[g % 2].name, k)])

            def load_x(i):
                S.dma('sp', lambda e: e.dma_start(out=xin[i % 3][:], in_=xsrc[i * 128:(i + 1) * 128, :]),
                      w=[xin[i % 3]])
            tmp2 = self.sb(es, "wtmp2", [128, D])
            deferred = []

            def s1(i):
                b = i // (NT // NSEQ)
                sub, g = i % 4, i // 4
                mxg = mx[g % 2]
                xi, x1i = xin[i % 3], x1[i % 2]
                for nh in range(2):
                    hs = slice(nh * 512, (nh + 1) * 512)
                    p = pb[nh]
                    for k in range(8):
                        S.op('pe', lambda e: e.matmul(p[:], lhsT=mxg[:, k, sub * 128:(sub + 1) * 128], rhs=wout[:, k, hs],
                                                      start=(k == 0), stop=(k == 7)), r=[(mxg.name, k), wout], w=[p])
                    S.op('dve', lambda e: e.tensor_tensor(out=tmp[:, hs], in0=p[:], in1=bcl[:, b, 0, hs], op=ALU.mult),
                         r=[p, bcl], w=[(tmp.name, nh)])
                    S.op('dve', lambda e: e.tensor_tensor(out=x1i[:, hs], in0=tmp[:, hs], in1=xi[:, hs], op=ALU.add),
                         r=[(tmp.name, nh), xi], w=[(x1i.name, nh)])
                deferred.append(lambda: S.dma(
                    'sp', lambda e: e.dma_start(out=self.xres[i * 128:(i + 1) * 128, :], in_=x1i[:]),
                    r=[(x1i.name, 0), (x1i.name, 1)], w=[("xres", i)]))

            def s2(i):
                b = i // (NT // NSEQ)
                x1i, h2bi = x1[i % 2], h2b[i % 2]
                S.raw('act', r=[(x1i.name, 0), (x1i.name, 1)])
                self.rms_rstd(x1i, junk, ss, rstd, D)
                S.op('dve', lambda e: e.scalar_tensor_tensor(out=tmp2[:], in0=x1i[:], scalar=rstd[:, 0:1],
                                                             in1=bcl[:, b, 2, :], op0=ALU.mult, op1=ALU.mult),
                     r=[(x1i.name, 0), (x1i.name, 1), rstd, bcl], w=[tmp2])
                S.op('dve', lambda e: e.tensor_tensor(out=h2[:], in0=tmp2[:], in1=bcl[:, b, 1, :], op=ALU.add),
                     r=[tmp2, bcl], w=[h2])
                S.op('act', lambda e: e.copy(out=h2bi[:], in_=h2[:]), r=[h2], w=[h2bi])
                deferred.append(lambda: S.dma(
                    'sp', lambda e: e.dma_start(out=self.H2[i * 128:(i + 1) * 128, :], in_=h2bi[:]), r=[h2bi],
                    w=[("H2", i)]))
                for hh in range(2):
                    pv = pb[2 + hh][:].rearrange("p (k t) -> p k t", k=4)
                    for k in range(4):
                        kk = hh * 4 + k
                        S.op('pe', lambda e: e.transpose(pv[:, k, :], h2[:, kk * 128:(kk + 1) * 128], self.ident_f),
                             r=[h2, self.cf], w=[pb[2 + hh]])
                    S.op('act', lambda e: e.copy(out=h2T[:, hh * 4:(hh + 1) * 4, :], in_=pv), r=[pb[2 + hh]],
                         w=[(h2T.name, hh)])
                for k in range(8):
                    S.op('pe', lambda e: e.matmul(pb[4][:, 0:36], lhsT=h2T[:, k, :], rhs=rw[:, k, :], start=(k == 0),
                                                  stop=False), r=[(h2T.name, k // 4), rw], w=[pb[4]])
                S.op('pe', lambda e: e.matmul(pb[4][:, 0:36], lhsT=self.ones_f[0:1, :], rhs=rb[0:1, :], start=False,
                                              stop=True), r=[self.cf, rb], w=[pb[4]])
                S.op('dve', lambda e: e.tensor_copy(out=RL[:, i, :], in_=pb[4][:, 0:36]), r=[pb[4]], w=[RL])

            load_mx(0)
            load_x(0)
            load_x(1)
            s1(0)
            for i in range(NT):
                if i + 2 < NT:
                    load_x(i + 2)
                if (i + 1) % 4 == 1 and (i + 1) // 4 + 1 < NT // 4:
                    load_mx((i + 1) // 4 + 1)
                for fn in deferred:
                    fn()
                deferred.clear()
                if i + 1 < NT:
                    s1(i + 1)
                s2(i)
            for fn in deferred:
                fn()
            V = 'dve'
            sm = self.sb(es, "rsm", [128, 8, NT])
            g4 = self.sb(es, "rg4", [128, NT, 4])
            ohg = self.sb(es, "ohg", [128, NT, 4])
            t48 = self.sb(es, "rt48", [128, NT, 4, 8])
            esel = self.sb(es, "esel", [128, NT, 8])
            esel2 = self.sb(es, "esel2", [128, NT, 8])
            oh = self.sb(es, "oh12", [128, 2, NT, 8])
            Eb = self.sb(es, "Eb", [128, NT * 64], BF16)
            pfx = self.sb(es, "rpfx", [128, NT, 64])
            tot = self.sb(es, "rtot", [128, NT, 64])
            ts_ = [self.sb(es, f"rts{i}", [128, NT, 32]) for i in range(2)]
            tsum = self.sb(es, "rtsum", [128, NT, 32])
            sc_ = [self.sb(es, f"scn{i}", [128, 32]) for i in range(2)]
            pci = self.sb(es, "pci", [128, 32], I32)
            pstart = self.sb(es, "pstart", [128, 32])
            bacc = self.sb(es, "bacc", [128, NBLK])
            boob = self.sb(es, "boob", [128, NBLK])
            dtmp = self.sb(es, "dtmp", [128, NT, 2, 32])
            dst = self.sb(es, "dstf", [128, NT, 2])
            lg = RL[:, :, 0:4]
            le = RL[:, :, 4:36].rearrange("p t (g e) -> p t g e", g=4)

            def bc3(ap, n):
                return ap.unsqueeze(2).to_broadcast([128, NT, n])
            S.op(V, lambda e: e.tensor_reduce(out=sm[:, 0, :], in_=lg, axis=AX.X, op=ALU.max), r=[RL], w=[sm])
            S.op(V, lambda e: e.tensor_tensor(out=ohg[:], in0=lg, in1=bc3(sm[:, 0, :], 4), op=ALU.is_equal),
                 r=[RL, sm], w=[ohg])
            S.op(V, lambda e: e.tensor_tensor(out=g4[:], in0=lg, in1=bc3(sm[:, 0, :], 4), op=ALU.subtract),
                 r=[RL, sm], w=[g4])
            S.op('act', lambda e: e.activation(out=g4[:], in_=g4[:], func=AF.Exp), r=[g4], w=[g4])
            S.op(V, lambda e: e.tensor_reduce(out=sm[:, 1, :], in_=g4[:], axis=AX.X, op=ALU.add), r=[g4], w=[sm])
            S.op(V, lambda e: e.reciprocal(out=sm[:, 2, :], in_=sm[:, 1, :]), r=[sm], w=[sm])
            S.op(V, lambda e: e.tensor_tensor(out=t48[:], in0=le, in1=ohg[:].unsqueeze(3).to_broadcast([128, NT, 4, 8]),
                                              op=ALU.mult), r=[RL, ohg], w=[t48])
            S.op(V, lambda e: e.tensor_reduce(out=esel[:], in_=t48[:].rearrange("p t g e -> p t e g"), axis=AX.X,
                                              op=ALU.add), r=[t48], w=[esel])
            S.op(V, lambda e: e.tensor_reduce(out=sm[:, 3, :], in_=esel[:], axis=AX.X, op=ALU.max), r=[esel], w=[sm])
            S.op(V, lambda e: e.tensor_tensor(out=oh[:, 0, :, :], in0=esel[:], in1=bc3(sm[:, 3, :], 8), op=ALU.is_equal),
                 r=[esel, sm], w=[oh])
            S.op(V, lambda e: e.scalar_tensor_tensor(out=esel2[:], in0=oh[:, 0, :, :], scalar=-1e30, in1=esel[:],
                                                     op0=ALU.mult, op1=ALU.add), r=[oh, esel], w=[esel2])
            S.op(V, lambda e: e.tensor_reduce(out=sm[:, 4, :], in_=esel2[:], axis=AX.X, op=ALU.max), r=[esel2], w=[sm])
            S.op(V, lambda e: e.tensor_tensor(out=oh[:, 1, :, :], in0=esel2[:], in1=bc3(sm[:, 4, :], 8),
                                              op=ALU.is_equal), r=[esel2, sm], w=[oh])
            S.op(V, lambda e: e.tensor_tensor(out=sm[:, 5, :], in0=sm[:, 3, :], in1=sm[:, 4, :], op=ALU.subtract),
                 r=[sm], w=[sm])
            S.op('act', lambda e: e.activation(out=sm[:, 6, :], in_=sm[:, 5, :], func=AF.Sigmoid), r=[sm], w=[sm])
            S.op(V, lambda e: e.tensor_tensor(out=self.GT[:, :, 0], in0=sm[:, 6, :], in1=sm[:, 2, :], op=ALU.mult),
                 r=[sm], w=["GT"])
            S.op(V, lambda e: e.tensor_tensor(out=self.GT[:, :, 1], in0=sm[:, 2, :], in1=self.GT[:, :, 0],
                                              op=ALU.subtract), r=[sm, "GT"], w=["GT"])
            Ev = self.EALL[:].rearrange("p t (k g e) -> p t k g e", k=2, g=4)
            for k in range(2):
                S.op(V, lambda e: e.tensor_tensor(out=Ev[:, :, k, :, :],
                                                  in0=ohg[:].unsqueeze(3).to_broadcast([128, NT, 4, 8]),
                                                  in1=oh[:, k, :, :].unsqueeze(2).to_broadcast([128, NT, 4, 8]),
                                                  op=ALU.mult), r=[ohg, oh], w=["EALL"])
            S.op('act', lambda e: e.copy(out=Eb[:], in_=self.EALL[:].rearrange("p t c -> p (t c)")), r=["EALL"], w=[Eb])
            for j in range(4):
                S.op('pe', lambda e: e.matmul(pb[j][:], lhsT=self.triu_b, rhs=Eb[:, j * 512:(j + 1) * 512], start=True,
                                              stop=True), r=[self.cb, Eb], w=[pb[j]])
                S.op('pe', lambda e: e.matmul(pb[4 + j][:], lhsT=self.ones_b, rhs=Eb[:, j * 512:(j + 1) * 512],
                                              start=True, stop=True), r=[self.cb, Eb], w=[pb[4 + j]])
                S.op('act', lambda e: e.copy(out=pfx[:, j * 8:(j + 1) * 8, :].rearrange("p t c -> p (t c)"),
                                             in_=pb[j][:]), r=[pb[j]], w=[pfx])
                S.op(V, lambda e: e.tensor_copy(out=tot[:, j * 8:(j + 1) * 8, :].rearrange("p t c -> p (t c)"),
                                                in_=pb[4 + j][:]), r=[pb[4 + j]], w=[tot])
            S.op(V, lambda e: e.tensor_tensor(out=tsum[:], in0=tot[:, :, 0:32], in1=tot[:, :, 32:64], op=ALU.add),
                 r=[tot], w=[tsum])
            S.op(V, lambda e: e.tensor_copy(out=ts_[0][:], in_=tsum[:]), r=[tsum], w=[ts_[0]])
            cur = 0
            shf = 1
            while shf < NT:
                a, bb = ts_[cur], ts_[1 - cur]
                S.op(V, lambda e: e.tensor_copy(out=bb[:, 0:shf, :], in_=a[:, 0:shf, :]), r=[a], w=[bb])
                S.op(V, lambda e: e.tensor_tensor(out=bb[:, shf:NT, :], in0=a[:, shf:NT, :], in1=a[:, 0:NT - shf, :],
                                                  op=ALU.add), r=[a, bb], w=[bb])
                cur = 1 - cur
                shf *= 2
            incl = ts_[cur]
            base = ts_[1 - cur]
            S.op(V, lambda e: e.tensor_tensor(out=base[:], in0=incl[:], in1=tsum[:], op=ALU.subtract),
                 r=[incl, tsum], w=[base])
            for k in range(2):
                S.op(V, lambda e: e.tensor_tensor(out=pfx[:, :, k * 32:(k + 1) * 32], in0=pfx[:, :, k * 32:(k + 1) * 32],
                                                  in1=base[:], op=ALU.add), r=[pfx, base], w=[pfx])
            S.op(V, lambda e: e.tensor_tensor(out=pfx[:, :, 32:64], in0=pfx[:, :, 32:64], in1=tot[:, :, 0:32],
                                              op=ALU.add), r=[pfx, tot], w=[pfx])
            S.op(V, lambda e: e.tensor_tensor(out=pfx[:], in0=pfx[:], in1=self.EALL[:], op=ALU.mult),
                 r=[pfx, "EALL"], w=[pfx])
            S.op(V, lambda e: e.tensor_reduce(out=self.RK[:], in_=pfx[:].rearrange("p t (k e) -> p t k e", k=2),
                                              axis=AX.X, op=ALU.add), r=[pfx], w=["RK"])
            sh = MB.bit_length() - 1
            S.op(V, lambda e: e.tensor_scalar(out=sc_[0][:], in0=incl[:, NT - 1, :], scalar1=float(MB - 1),
                                              scalar2=None, op0=ALU.add), r=[incl], w=[sc_[0]])
            S.op(V, lambda e: e.tensor_copy(out=pci[:], in_=sc_[0][:]), r=[sc_[0]], w=[pci])
            S.op(V, lambda e: e.tensor_scalar(out=pci[:], in0=pci[:], scalar1=sh, scalar2=sh,
                                              op0=ALU.arith_shift_right, op1=ALU.logical_shift_left), r=[pci], w=[pci])
            S.op(V, lambda e: e.tensor_copy(out=sc_[0][:], in_=pci[:]), r=[pci], w=[sc_[0]])
            S.op(V, lambda e: e.tensor_copy(out=pstart[:], in_=sc_[0][:]), r=[sc_[0]], w=[pstart])
            cur = 0
            for shf in (1, 2, 4, 8, 16):
                a, bb = sc_[cur], sc_[1 - cur]
                S.op(V, lambda e: e.tensor_copy(out=bb[:, 0:shf], in_=a[:, 0:shf]), r=[a], w=[bb])
                S.op(V, lambda e: e.tensor_tensor(out=bb[:, shf:32], in0=a[:, shf:32], in1=a[:, 0:32 - shf], op=ALU.add),
                     r=[a, bb], w=[bb])
                cur = 1 - cur
            pend = sc_[cur]
            S.op(V, lambda e: e.tensor_tensor(out=pstart[:], in0=pend[:], in1=pstart[:], op=ALU.subtract),
                 r=[pend, pstart], w=[pstart])
            S.op('pool', lambda e: e.memset(bacc[:], 0.0), w=[bacc])
            for ex in range(NEXP):
                S.op(V, lambda e: e.scalar_tensor_tensor(out=bacc[:], in0=self.cf[:, 512:512 + NBLK],
                                                         scalar=pend[:, ex:ex + 1], in1=bacc[:], op0=ALU.is_ge,
                                                         op1=ALU.add), r=[self.cf, pend, bacc], w=[bacc])
            S.op(V, lambda e: e.tensor_scalar(out=boob[:], in0=bacc[:], scalar1=float(NEXP), scalar2=1.0e6,
                                              op0=ALU.is_ge, op1=ALU.mult), r=[bacc], w=[boob])
            S.op(V, lambda e: e.tensor_scalar(out=bacc[:], in0=bacc[:], scalar1=float(NEXP - 1), scalar2=None,
                                              op0=ALU.min), r=[bacc], w=[bacc])
            S.op(V, lambda e: e.tensor_scalar(out=bacc[:], in0=bacc[:], scalar1=128.0, scalar2=self.cf[:, 256:257],
                                              op0=ALU.mult, op1=ALU.add), r=[bacc, self.cf], w=[bacc])
            S.op(V, lambda e: e.tensor_tensor(out=bacc[:], in0=bacc[:], in1=boob[:], op=ALU.add), r=[bacc, boob],
                 w=[bacc])
            S.op(V, lambda e: e.tensor_copy(out=self.BEXP[:], in_=bacc[:]), r=[bacc], w=[self.BEXP])
            S.op(V, lambda e: e.tensor_tensor(out=dtmp[:], in0=self.EALL[:].rearrange("p t (k e) -> p t k e", k=2),
                                              in1=pstart[:].unsqueeze(1).unsqueeze(1).to_broadcast([128, NT, 2, 32]),
                                              op=ALU.mult), r=["EALL", pstart], w=[dtmp])
            S.op(V, lambda e: e.tensor_reduce(out=dst[:], in_=dtmp[:], axis=AX.X, op=ALU.add), r=[dtmp], w=[dst])
            S.op(V, lambda e: e.tensor_tensor(out=dst[:], in0=dst[:], in1=self.RK[:], op=ALU.add), r=[dst, "RK"],
                 w=[dst])
            S.op(V, lambda e: e.tensor_copy(out=self.DSTI[:], in_=dst[:]), r=[dst], w=["DSTI"])
            S.raw('pool', r=zkeys)
            h2s = [self.sb(es, f"wh2s{i}", [128, D], BF16) for i in range(6)]
            for i in range(NT):
                h2bi = h2s[i % 6]
                S.dma('sp', lambda e: e.dma_start(out=h2bi[:], in_=self.H2[i * 128:(i + 1) * 128, :]), r=[("H2", i)],
                      w=[h2bi])
                for k in range(2):
                    S.dma('pool', lambda e: e.indirect_dma_start(
                        out=self.Hs[:, :], out_offset=bass.IndirectOffsetOnAxis(ap=self.DSTI[:, i, k:k + 1], axis=0),
                        in_=h2bi[:], in_offset=None), r=[h2bi, "DSTI"], w=[("Hs", i, k)])
        S.barrier()

    def phase_experts(self, l):
        S, nc = self.S, self.nc
        NSUB = MB // 128
        with ExitStack() as es:
            pb = [self.ps(es, f"eb{i}", [128, 512]) for i in range(8)]
            w1b = [self.sb(es, f"w1b{i}", [128, 8, DEXP], BF16) for i in range(3)]
            w3b = [self.sb(es, f"w3b{i}", [128, 8, DEXP], BF16) for i in range(3)]
            w2b = [self.sb(es, f"w2b{i}", [128, 4, D], BF16) for i in range(3)]
            hb = [self.sb(es, f"ehb{i}", [128, NSUB, D], BF16) for i in range(2)]
            hbT = [self.sb(es, f"ehbT{i}", [128, 8, MB], BF16) for i in range(2)]
            sa = [self.sb(es, f"esa{i}", [128, MB]) for i in range(2)]
            abT = [self.sb(es, f"eabT{i}", [128, 4, MB], BF16) for i in range(2)]
            ysb = [self.sb(es, f"eys{i}", [128, D]) for i in range(4)]
            ny = 0
            npp = 0
            deferred = []
            if not hasattr(self, "oob_reg"):
                self.oob_reg = nc.gpsimd.to_reg(NEXP * 128 - 1)
            for bk in range(NBLK):
                pi = bk % 2
                wi3 = bk % 3
                for wsrc, wdst, kh in ((self.exp_w1, w1b[wi3], 4), (self.exp_w3, w3b[wi3], 4), (self.exp_w2, w2b[wi3], 2)):
                    for h in range(2):
                        S.dma('pool', lambda e: e.indirect_dma_start(
                            out=wdst[:, h * kh:(h + 1) * kh, :].rearrange("p k f -> p (k f)"), out_offset=None,
                            in_=wsrc[l][h][:, :],
                            in_offset=bass.IndirectOffsetOnAxis(ap=self.BEXP[:, bk:bk + 1], axis=0),
                            bounds_check=self.oob_reg, oob_is_err=False),
                            r=[self.BEXP], w=[(wdst.name, h)])
                if bk == 0:
                    S.dma('sp', lambda e: e.dma_start(
                        out=hb[0][:], in_=self.Hs[0:MB, :].rearrange("(s p) d -> p s d", p=128)), r=["Hs"], w=[hb[0]])
                if bk + 1 < NBLK:
                    S.dma('sp', lambda e: e.dma_start(
                        out=hb[1 - pi][:],
                        in_=self.Hs[(bk + 1) * MB:(bk + 2) * MB, :].rearrange("(s p) d -> p s d", p=128)),
                        r=["Hs"], w=[hb[1 - pi]])
                for fn in deferred:
                    fn()
                deferred = []
                def tr(bb):
                    pj = bb % 2
                    for sub in range(NSUB):
                        tpv = pb[sub][:].bitcast(BF16).rearrange("p (k t) -> p k t", k=8)
                        for k in range(8):
                            S.op('pe', lambda e: e.transpose(tpv[:, k, :], hb[pj][:, sub, k * 128:(k + 1) * 128],
                                                             self.ident_b), r=[hb[pj], self.cb], w=[pb[sub]])
                        S.op('act', lambda e: e.copy(out=hbT[pj][:, :, sub * 128:(sub + 1) * 128], in_=tpv),
                             r=[pb[sub]], w=[(hbT[pj].name, sub)])
                if bk == 0:
                    tr(0)
                for fc in range(4):
                    pa, pg = pb[2 + (npp % 2) * 2], pb[3 + (npp % 2) * 2]
                    sai = sa[npp % 2]
                    npp += 1
                    for k in range(8):
                        S.op('pe', lambda e: e.matmul(pa[:, 0:MB], lhsT=w1b[wi3][:, k, fc * 128:(fc + 1) * 128],
                                                      rhs=hbT[pi][:, k, :], start=(k == 0), stop=(k == 7)),
                             r=[(w1b[wi3].name, 0), (w1b[wi3].name, 1), (hbT[pi].name, 0), (hbT[pi].name, 1)], w=[pa])
                    for k in range(8):
                        S.op('pe', lambda e: e.matmul(pg[:, 0:MB], lhsT=w3b[wi3][:, k, fc * 128:(fc + 1) * 128],
                                                      rhs=hbT[pi][:, k, :], start=(k == 0), stop=(k == 7)),
                             r=[(w3b[wi3].name, 0), (w3b[wi3].name, 1), (hbT[pi].name, 0), (hbT[pi].name, 1)], w=[pg])
                    S.op('act', lambda e: e.activation(out=sai[:], in_=pa[:, 0:MB], func=AF.Silu), r=[pa], w=[sai])
                    S.op('dve', lambda e: e.tensor_tensor(out=abT[pi][:, fc, :], in0=pg[:, 0:MB], in1=sai[:], op=ALU.mult),
                         r=[pg, sai], w=[(abT[pi].name, fc)])
                if bk + 1 < NBLK:
                    tr(bk + 1)
                for sub in range(NSUB):
                    y = ysb[ny % 4]
                    ny += 1
                    for nh in range(2):
                        p = pb[6 + nh]
                        for kf in range(4):
                            S.op('pe', lambda e: e.matmul(p[:], lhsT=abT[pi][:, kf, sub * 128:(sub + 1) * 128],
                                                          rhs=w2b[wi3][:, kf, nh * 512:(nh + 1) * 512], start=(kf == 0),
                                                          stop=(kf == 3)), r=[(abT[pi].name, kf), (w2b[wi3].name, 0), (w2b[wi3].name, 1)], w=[p])
                        if nh == 0:
                            S.op('act', lambda e: e.copy(out=y[:, 0:512], in_=p[:]), r=[p], w=[(y.name, 0)])
                        else:
                            S.op('dve', lambda e: e.tensor_copy(out=y[:, 512:1024], in_=p[:]), r=[p], w=[(y.name, 1)])
                    r0 = bk * MB + sub * 128
                    deferred.append(lambda y=y, r0=r0, bk=bk, sub=sub: S.dma(
                        'sp', lambda e: e.dma_start(out=self.Ys[r0:r0 + 128, :], in_=y[:]),
                        r=[(y.name, 0), (y.name, 1)], w=[("Ys", bk, sub)]))
            for fn in deferred:
                fn()
        S.barrier()

    def phase_combine(self, l, last):
        S, nc = self.S, self.nc
        NB = 4
        with ExitStack() as es:
            g2 = self.sb(es, "g2bc", [128, NSEQ, D])
            fg = self.sb(es, "fgbc", [128, D])
            xin = [self.sb(es, f"cx{i}", [128, D]) for i in range(NB)]
            y0 = [self.sb(es, f"cy0{i}", [128, D]) for i in range(NB)]
            y1 = [self.sb(es, f"cy1{i}", [128, D]) for i in range(NB)]
            mo = [self.sb(es, f"cmo{i}", [128, D]) for i in range(2)]
            x2 = [self.sb(es, f"cx2{i}", [128, D]) for i in range(NB)]
            junk = self.sb(es, "cjunk", [128, D], BF16)
            ss = [self.sb(es, f"css{i}", [128, 1]) for i in range(2)]
            rstd = [self.sb(es, f"crstd{i}", [128, 1]) for i in range(2)]
            for b in range(NSEQ):
                S.dma('sp', lambda e: e.dma_start(out=g2[:, b, :], in_=self.bcD[:, b, 5, :]), w=[g2])
            if last:
                S.dma('sp', lambda e: e.dma_start(out=fg[:], in_=self.final_g[0:1, :].partition_broadcast(128)), w=[fg])

            def fetch(i):
                S.dma('sp', lambda e: e.dma_start(out=xin[i % NB][:], in_=self.xres[i * 128:(i + 1) * 128, :]),
                      r=[("xres", i)], w=[xin[i % NB]])
                for k, yy in ((0, y0[i % NB]), (1, y1[i % NB])):
                    S.dma('pool', lambda e: e.indirect_dma_start(
                        out=yy[:], out_offset=None, in_=self.Ys[:, :],
                        in_offset=bass.IndirectOffsetOnAxis(ap=self.DSTI[:, i, k:k + 1], axis=0)), r=["DSTI"], w=[yy])
            fetch(0)
            fetch(1)
            deferred = []
            for i in range(NT):
                b = i // (NT // NSEQ)
                if i + 2 < NT:
                    fetch(i + 2)
                for fn in deferred:
                    fn()
                deferred = []
                xi, y0i, y1i, x2i, moi = xin[i % NB], y0[i % NB], y1[i % NB], x2[i % NB], mo[i % 2]
                S.op('act', lambda e: e.mul(out=moi[:], in_=y0i[:], mul=self.GT[:, i, 0:1]), r=[y0i, "GT"], w=[moi])
                S.op('dve', lambda e: e.scalar_tensor_tensor(out=moi[:], in0=y1i[:], scalar=self.GT[:, i, 1:2], in1=moi[:],
                                                             op0=ALU.mult, op1=ALU.add), r=[y1i, moi, "GT"], w=[moi])
                S.op('dve', lambda e: e.tensor_tensor(out=moi[:], in0=moi[:], in1=g2[:, b, :], op=ALU.mult),
                     r=[moi, g2], w=[moi])
                S.op('dve', lambda e: e.tensor_tensor(out=x2i[:], in0=moi[:], in1=xi[:], op=ALU.add), r=[moi, xi],
                     w=[x2i])
                if not last:
                    deferred.append(lambda i=i, x2i=x2i: S.dma(
                        'sp', lambda e: e.dma_start(out=self.xres[i * 128:(i + 1) * 128, :], in_=x2i[:]), r=[x2i],
                        w=[("xres", i)]))
                else:
                    self.rms_rstd(x2i, junk, ss[i % 2], rstd[i % 2], D)
                    rs = rstd[i % 2]
                    S.op('dve', lambda e: e.scalar_tensor_tensor(out=x2i[:], in0=x2i[:], scalar=rs[:, 0:1], in1=fg[:],
                                                                 op0=ALU.mult, op1=ALU.mult), r=[x2i, rs, fg],
                         w=[x2i])
                    deferred.append(lambda i=i, x2i=x2i: S.dma(
                        'sp', lambda e: e.dma_start(out=self.out[i * 128:(i + 1) * 128, :], in_=x2i[:]), r=[x2i],
                        w=[("out", i)]))
            for fn in deferred:
                fn()
        S.barrier()

    def build_all(self):
        self.declare()
        self.setup()
        for l in range(self.nlayers):
            self.phase_mod(l)
            self.phase_front(l)
            self.phase_conv(l)
            self.phase_pool(l)
            self.phase_attn(l)
            self.phase_wout(l)
            self.phase_experts(l)
            self.phase_combine(l, last=(l == self.nlayers - 1))
        self.S.barrier()
        self.es.close()
        return self.nc


def _wlay(w, kh):
    w = np.asarray(w, dtype=np.float32)
    L, E, R, Fd = w.shape
    w = w.reshape(L, E, 2, kh, 128, Fd).transpose(0, 2, 1, 4, 3, 5)
    return np.ascontiguousarray(w).reshape(L, 2, E * 128, kh * Fd)


def host_inputs(inputs):
    f = lambda a: np.ascontiguousarray(np.asarray(a, dtype=np.float32))
    cf, cb, pinv = make_consts()
    L = DEPTH
    shared = {
        "mod_w": f(inputs["mod_w"]), "mod_b": f(inputs["mod_b"]), "norm1_g": f(inputs["norm1_g"]),
        "w_in": f(inputs["w_in"]), "conv_k": f(inputs["conv_k"]), "conv_b": f(inputs["conv_b"]),
        "conv_ln_g": f(inputs["conv_ln_g"]), "conv_ln_b": f(inputs["conv_ln_b"]),
        "q_norm_g": f(inputs["q_norm_g"]), "kv_norm_g": f(inputs["kv_norm_g"]),
        "w_uq": f(inputs["w_uq"]).reshape(L, 256, 512), "w_uk": f(inputs["w_uk"]).reshape(L, 128, 512),
        "w_uv": f(inputs["w_uv"]).reshape(L, 128, 512), "pool_w": f(inputs["pool_w"]),
        "pool_scale": f(inputs["pool_scale"]), "w_out": f(inputs["w_out"]), "norm2_g": f(inputs["norm2_g"]),
        "router_w": np.ascontiguousarray(np.concatenate(
            [f(inputs["router_g_w"]), f(inputs["router_e_w"]).reshape(L, D, 32)], axis=2)),
        "router_b": np.ascontiguousarray(np.concatenate(
            [f(inputs["router_g_b"]), f(inputs["router_e_b"]).reshape(L, 32)], axis=1)),
        "final_g": f(inputs["final_g"]).reshape(1, D), "cf": cf, "cb": cb, "pinv": pinv,
    }
    for nm, kh in (("exp_w1", 4), ("exp_w3", 4), ("exp_w2", 2)):
        wl = _wlay(inputs[nm], kh)
        for a in range(L):
            for h in range(2):
                shared[f"{nm}_{a}_{h}"] = np.ascontiguousarray(wl[a, h])
    x = f(inputs["x"])
    c = f(inputs["c"])
    maps = []
    for i in range(NCORES):
        m = dict(shared)
        m["x"] = np.ascontiguousarray(x[i * NSEQ:(i + 1) * NSEQ].reshape(T, D))
        m["c"] = np.ascontiguousarray(c[i * NSEQ:(i + 1) * NSEQ])
        maps.append(m)
    return maps


_CACHE = {}


def kernel(**inputs):
    maps = host_inputs(inputs)
    if "nc" not in _CACHE:
        _CACHE["nc"] = Prog().build_all()
    res = run_bass_kernel_spmd(_CACHE["nc"], maps, core_ids=list(range(NCORES)))
    outs = [np.asarray(r["out"], dtype=np.float32).reshape(NSEQ, SEQ, D) for r in res.results]
    return np.concatenate(outs, axis=0)
```
